# Optimizing a Trainium2 kernel written in Bass

```python
import math
import jax
import jax.numpy as jnp
from jax import lax
import numpy as np


D_MODEL = 1024
BATCH = 4
SEQ = 4096
DEPTH = 4

HEAD_DIM = 64
N_HEADS_A = 8
N_HEADS_B = 8
N_HEADS_C = 8
N_HEADS_D = 8
MOBA_BLOCK = 256
MOBA_TOPK = 3
MOBA_Q_CHUNK = 32
DILATED_BRANCHES = ((128, 1), (512, 4), (2048, 16))
Q_BLOCK = 128
MLA_Q_RANK = 256
MLA_KV_RANK = 256
MLA_NOPE_DIM = 64
MLA_ROPE_DIM = 32
MLA_V_DIM = 64
ROPE_THETA = 10000.0
REL_BUCKETS = 32
REL_MAX_DIST = 2048
D_FF = 2816
CONV_WIDTH = 3
NORM_EPS = 1e-6
N_EVEN = (DEPTH + 1) // 2
N_ODD = DEPTH // 2
AB_IN = 3 * HEAD_DIM * (N_HEADS_A + N_HEADS_B)
AB_OUT = HEAD_DIM * (N_HEADS_A + N_HEADS_B)
CD_IN = MLA_Q_RANK + MLA_KV_RANK + MLA_ROPE_DIM + 3 * HEAD_DIM * N_HEADS_D
CD_OUT = MLA_V_DIM * N_HEADS_C + HEAD_DIM * N_HEADS_D

kernel_name = 'hybrid_moba_dilated_mla_stickbreaking'


def rmsnorm(x, g):
    x32 = x.astype(jnp.float32)
    y = x32 * lax.rsqrt(jnp.mean(x32 * x32, axis=-1, keepdims=True) + NORM_EPS)
    return (y * g.astype(jnp.float32)).astype(x.dtype)


def t5_bucket(dist):
    dist = jnp.maximum(dist, 0)
    max_exact = REL_BUCKETS // 2
    d_f = jnp.maximum(dist, max_exact).astype(jnp.float32)
    large = max_exact + (jnp.log(d_f / max_exact) / math.log(REL_MAX_DIST / max_exact)
                         * (REL_BUCKETS - max_exact)).astype(jnp.int32)
    large = jnp.minimum(large, REL_BUCKETS - 1)
    return jnp.where(dist < max_exact, dist, large)


def split_cols(t, sizes):
    return jnp.split(t, [int(o) for o in np.cumsum(sizes)[:-1]], axis=-1)


def to_heads(t, n_heads):
    b, s, _ = t.shape
    return t.reshape(b, s, n_heads, -1).transpose(0, 2, 1, 3)


def from_heads(t):
    b, h, s, d = t.shape
    return t.transpose(0, 2, 1, 3).reshape(b, s, h * d)


def query_blocks(q, qb):
    b, h, s, d = q.shape
    return q.reshape(b, h, s // qb, qb, d).transpose(2, 0, 1, 3, 4)


def merge_blocks(o):
    n, b, h, qb, d = o.shape
    return o.transpose(1, 2, 0, 3, 4).reshape(b, h, n * qb, d)


def rope_tables(s):
    inv_freq = 1.0 / (ROPE_THETA ** (jnp.arange(0, MLA_ROPE_DIM, 2, dtype=jnp.float32) / MLA_ROPE_DIM))
    ang = jnp.arange(s, dtype=jnp.float32)[:, None] * inv_freq[None, :]
    return jnp.cos(ang), jnp.sin(ang)


def apply_rope(x, cos, sin):
    cos = cos[:, None, :].astype(x.dtype)
    sin = sin[:, None, :].astype(x.dtype)
    x1, x2 = jnp.split(x, 2, axis=-1)
    return jnp.concatenate([x1 * cos - x2 * sin, x1 * sin + x2 * cos], axis=-1)


def moba_attention(q, k, v, bias_table):
    b, h, s, dh = q.shape
    nb = -(-s // MOBA_BLOCK)
    pad = nb * MOBA_BLOCK - s
    kp = jnp.pad(k, ((0, 0), (0, 0), (0, pad), (0, 0)))
    vp = jnp.pad(v, ((0, 0), (0, 0), (0, pad), (0, 0)))
    kb = kp.reshape(b, h, nb, MOBA_BLOCK, dh)
    vb = vp.reshape(b, h, nb, MOBA_BLOCK, dh)
    k_mean = jnp.mean(kb, axis=3)
    scale = dh ** -0.5
    topk = min(MOBA_TOPK, nb)
    head_idx = jnp.arange(h)[:, None, None, None]
    blk_offsets = jnp.arange(MOBA_BLOCK)
    gather_blocks = jax.vmap(jax.vmap(lambda arr, idx: arr[idx]))

    def chunk_fn(args):
        qc, ci = args
        t = ci * MOBA_Q_CHUNK + jnp.arange(MOBA_Q_CHUNK)
        own = t[0] // MOBA_BLOCK
        gate = jnp.einsum('bhqd,bhnd->bhqn', qc, k_mean).astype(jnp.float32)
        past = jnp.arange(nb)[None, :] < (t // MOBA_BLOCK)[:, None]
        gate = jnp.where(past, gate, -jnp.inf)
        _, sel = lax.top_k(gate, topk)
        sel_valid = sel < own
        k_sel = gather_blocks(kb, sel)
        v_sel = gather_blocks(vb, sel)
        pos_sel = sel[..., None] * MOBA_BLOCK + blk_offsets
        logit_sel = (jnp.einsum('bhqd,bhqkld->bhqkl', qc, k_sel).astype(jnp.float32) * scale
                     + bias_table[head_idx, t5_bucket(t[:, None, None] - pos_sel)].astype(jnp.float32))
        logit_sel = jnp.where(sel_valid[..., None], logit_sel, -jnp.inf)
        k_own = lax.dynamic_slice_in_dim(kp, own * MOBA_BLOCK, MOBA_BLOCK, axis=2)
        v_own = lax.dynamic_slice_in_dim(vp, own * MOBA_BLOCK, MOBA_BLOCK, axis=2)
        pos_own = own * MOBA_BLOCK + blk_offsets
        logit_own = (jnp.einsum('bhqd,bhld->bhql', qc, k_own).astype(jnp.float32) * scale
                     + bias_table[:, t5_bucket(t[:, None] - pos_own[None, :])].astype(jnp.float32))
        logit_own = jnp.where(pos_own[None, :] <= t[:, None], logit_own, -jnp.inf)
        logits = jnp.concatenate([logit_sel.reshape(b, h, MOBA_Q_CHUNK, topk * MOBA_BLOCK), logit_own], axis=-1)
        p = jax.nn.softmax(logits, axis=-1)
        p_sel = p[..., :topk * MOBA_BLOCK].reshape(b, h, MOBA_Q_CHUNK, topk, MOBA_BLOCK).astype(v.dtype)
        p_own = p[..., topk * MOBA_BLOCK:].astype(v.dtype)
        return (jnp.einsum('bhqkl,bhqkld->bhqd', p_sel, v_sel)
                + jnp.einsum('bhql,bhld->bhqd', p_own, v_own))

    n_chunks = s // MOBA_Q_CHUNK
    return merge_blocks(lax.map(chunk_fn, (query_blocks(q, MOBA_Q_CHUNK), jnp.arange(n_chunks))))


def dilated_attention(q, k, v, bias_table):
    b, h, s, dh = q.shape
    scale = dh ** -0.5

    def chunk_fn(args):
        qc, ci = args
        t = ci * Q_BLOCK + jnp.arange(Q_BLOCK)
        lses, outs = [], []
        for window, dil in DILATED_BRANCHES:
            dist = dil * jnp.arange(window // dil + 1)
            pos = t[:, None] - dist[None, :]
            valid = pos >= 0
            pos_c = jnp.maximum(pos, 0)
            k_g = k[:, :, pos_c]
            v_g = v[:, :, pos_c]
            logit = (jnp.einsum('bhqd,bhqmd->bhqm', qc, k_g).astype(jnp.float32) * scale
                     + bias_table[:, t5_bucket(dist)][:, None, :].astype(jnp.float32))
            logit = jnp.where(valid, logit, -jnp.inf)
            lse = jax.nn.logsumexp(logit, axis=-1, keepdims=True)
            p = jnp.exp(logit - lse).astype(v.dtype)
            outs.append(jnp.einsum('bhqm,bhqmd->bhqd', p, v_g))
            lses.append(lse)
        w = jax.nn.softmax(jnp.concatenate(lses, axis=-1), axis=-1).astype(v.dtype)
        return jnp.einsum('bhqr,rbhqd->bhqd', w, jnp.stack(outs))

    n_chunks = s // Q_BLOCK
    return merge_blocks(lax.map(chunk_fn, (query_blocks(q, Q_BLOCK), jnp.arange(n_chunks))))


def causal_softmax_attention(q, k, v):
    s = q.shape[2]
    scale = q.shape[-1] ** -0.5
    kpos = jnp.arange(s)

    def block_fn(args):
        qc, bi = args
        t = bi * Q_BLOCK + jnp.arange(Q_BLOCK)
        logits = jnp.einsum('bhqd,bhsd->bhqs', qc, k).astype(jnp.float32) * scale
        logits = jnp.where(kpos[None, :] <= t[:, None], logits, -jnp.inf)
        p = jax.nn.softmax(logits, axis=-1).astype(v.dtype)
        return jnp.einsum('bhqs,bhsd->bhqd', p, v)

    return merge_blocks(lax.map(block_fn, (query_blocks(q, Q_BLOCK), jnp.arange(s // Q_BLOCK))))


def mla_attention(c_q, c_kv, k_rope, q_norm_g, kv_norm_g, w_uq, w_ukv, cos, sin):
    b, s, _ = c_q.shape
    q = (rmsnorm(c_q, q_norm_g) @ w_uq).reshape(b, s, N_HEADS_C, MLA_NOPE_DIM + MLA_ROPE_DIM)
    kv = (rmsnorm(c_kv, kv_norm_g) @ w_ukv).reshape(b, s, N_HEADS_C, MLA_NOPE_DIM + MLA_V_DIM)
    q_nope, q_pe = q[..., :MLA_NOPE_DIM], q[..., MLA_NOPE_DIM:]
    k_nope, v = kv[..., :MLA_NOPE_DIM], kv[..., MLA_NOPE_DIM:]
    q_pe = apply_rope(q_pe, cos, sin)
    k_pe = apply_rope(k_rope[:, :, None, :], cos, sin)
    k_pe = jnp.broadcast_to(k_pe, (b, s, N_HEADS_C, MLA_ROPE_DIM))
    q_full = jnp.concatenate([q_nope, q_pe], axis=-1).transpose(0, 2, 1, 3)
    k_full = jnp.concatenate([k_nope, k_pe], axis=-1).transpose(0, 2, 1, 3)
    return causal_softmax_attention(q_full, k_full, v.transpose(0, 2, 1, 3))


def stick_breaking_attention(q, k, v):
    s = q.shape[2]
    scale = q.shape[-1] ** -0.5
    kpos = jnp.arange(s)

    def block_fn(args):
        qc, bi = args
        t = bi * Q_BLOCK + jnp.arange(Q_BLOCK)
        z = jnp.einsum('bhqd,bhsd->bhqs', qc, k).astype(jnp.float32) * scale
        strict = kpos[None, :] < t[:, None]
        log_keep = jnp.where(strict, jax.nn.log_sigmoid(-z), 0.0)
        rev = lax.cumsum(log_keep, axis=3, reverse=True)
        after = jnp.concatenate([rev[..., 1:], jnp.zeros_like(rev[..., :1])], axis=-1)
        a = jnp.where(strict, jnp.exp(jax.nn.log_sigmoid(z) + after), 0.0).astype(v.dtype)
        return jnp.einsum('bhqs,bhsd->bhqd', a, v)

    return merge_blocks(lax.map(block_fn, (query_blocks(q, Q_BLOCK), jnp.arange(s // Q_BLOCK))))


def even_mixer(h, w_in, w_out, rel_bias):
    proj = h @ w_in
    wa = N_HEADS_A * HEAD_DIM
    wb = N_HEADS_B * HEAD_DIM
    qa, ka, va, qb, kb, vb = split_cols(proj, [wa, wa, wa, wb, wb, wb])
    o_a = moba_attention(to_heads(qa, N_HEADS_A), to_heads(ka, N_HEADS_A), to_heads(va, N_HEADS_A),
                         rel_bias[:N_HEADS_A])
    o_b = dilated_attention(to_heads(qb, N_HEADS_B), to_heads(kb, N_HEADS_B), to_heads(vb, N_HEADS_B),
                            rel_bias[N_HEADS_A:])
    return jnp.concatenate([from_heads(o_a), from_heads(o_b)], axis=-1) @ w_out


def odd_mixer(h, w_in, q_norm_g, kv_norm_g, w_uq, w_ukv, w_out, cos, sin):
    proj = h @ w_in
    wd = N_HEADS_D * HEAD_DIM
    c_q, c_kv, k_rope, qd, kd, vd = split_cols(proj, [MLA_Q_RANK, MLA_KV_RANK, MLA_ROPE_DIM, wd, wd, wd])
    o_c = mla_attention(c_q, c_kv, k_rope, q_norm_g, kv_norm_g, w_uq, w_ukv, cos, sin)
    o_d = stick_breaking_attention(to_heads(qd, N_HEADS_D), to_heads(kd, N_HEADS_D), to_heads(vd, N_HEADS_D))
    return jnp.concatenate([from_heads(o_c), from_heads(o_d)], axis=-1) @ w_out


def conv_ffn(h, w_up, conv_w, conv_b, w_down):
    s = h.shape[1]
    u = h @ w_up
    up = jnp.pad(u, ((0, 0), (CONV_WIDTH - 1, 0), (0, 0)))
    u = sum(conv_w[j] * up[:, j:j + s] for j in range(CONV_WIDTH)) + conv_b
    gate, val = jnp.split(u, 2, axis=-1)
    return (jax.nn.gelu(gate, approximate=True) * val) @ w_down


def setup_inputs(seed: int = 0) -> dict:
    key = jax.random.key(seed)
    ks = jax.random.split(key, 24)
    f32 = jnp.float32

    def dense(k, shape, fan_in, gain=1.0):
        return jax.random.normal(k, shape, f32) * (gain * fan_in ** -0.5)

    def gain(k, shape):
        return 1.0 + 0.05 * jax.random.normal(k, shape, f32)

    return {
        'x': jax.random.normal(ks[0], (BATCH, SEQ, D_MODEL), f32),
        'c': jax.random.normal(ks[1], (BATCH, D_MODEL), f32),
        'rel_bias': 0.5 * jax.random.normal(ks[2], (N_HEADS_A + N_HEADS_B, REL_BUCKETS), f32),
        'ada_w': dense(ks[3], (DEPTH, D_MODEL, 6 * D_MODEL), D_MODEL, 0.5),
        'ada_b': 0.02 * jax.random.normal(ks[4], (DEPTH, 6 * D_MODEL), f32),
        'mix_pre_g': gain(ks[5], (DEPTH, D_MODEL)),
        'mix_post_g': gain(ks[6], (DEPTH, D_MODEL)),
        'ffn_pre_g': gain(ks[7], (DEPTH, D_MODEL)),
        'ffn_post_g': gain(ks[8], (DEPTH, D_MODEL)),
        'ab_w_in': dense(ks[9], (N_EVEN, D_MODEL, AB_IN), D_MODEL),
        'ab_w_out': dense(ks[10], (N_EVEN, AB_OUT, D_MODEL), AB_OUT),
        'cd_w_in': dense(ks[11], (N_ODD, D_MODEL, CD_IN), D_MODEL),
        'mla_q_norm_g': gain(ks[12], (N_ODD, MLA_Q_RANK)),
        'mla_kv_norm_g': gain(ks[13], (N_ODD, MLA_KV_RANK)),
        'mla_w_uq': dense(ks[14], (N_ODD, MLA_Q_RANK, N_HEADS_C * (MLA_NOPE_DIM + MLA_ROPE_DIM)), MLA_Q_RANK),
        'mla_w_ukv': dense(ks[15], (N_ODD, MLA_KV_RANK, N_HEADS_C * (MLA_NOPE_DIM + MLA_V_DIM)), MLA_KV_RANK),
        'cd_w_out': dense(ks[16], (N_ODD, CD_OUT, D_MODEL), CD_OUT),
        'ffn_w_up': dense(ks[17], (DEPTH, D_MODEL, 2 * D_FF), D_MODEL),
        'ffn_conv_w': dense(ks[18], (DEPTH, CONV_WIDTH, 2 * D_FF), CONV_WIDTH),
        'ffn_conv_b': 0.02 * jax.random.normal(ks[19], (DEPTH, 2 * D_FF), f32),
        'ffn_w_down': dense(ks[20], (DEPTH, D_FF, D_MODEL), D_FF),
    }


def reference(x, c, rel_bias, ada_w, ada_b, mix_pre_g, mix_post_g, ffn_pre_g, ffn_post_g,
              ab_w_in, ab_w_out, cd_w_in, mla_q_norm_g, mla_kv_norm_g, mla_w_uq, mla_w_ukv, cd_w_out,
              ffn_w_up, ffn_conv_w, ffn_conv_b, ffn_w_down):
    s = x.shape[1]
    cos, sin = rope_tables(s)
    cond = jax.nn.silu(c)
    for layer in range(DEPTH):
        mod = cond @ ada_w[layer] + ada_b[layer]
        shift_m, scale_m, gate_m, shift_f, scale_f, gate_f = [m[:, None, :] for m in jnp.split(mod, 6, axis=-1)]
        h = rmsnorm(x, mix_pre_g[layer]) * (1.0 + scale_m) + shift_m
        i = layer // 2
        if layer % 2 == 0:
            y = even_mixer(h, ab_w_in[i], ab_w_out[i], rel_bias)
        else:
            y = odd_mixer(h, cd_w_in[i], mla_q_norm_g[i], mla_kv_norm_g[i], mla_w_uq[i], mla_w_ukv[i],
                          cd_w_out[i], cos, sin)
        x = x + gate_m * rmsnorm(y, mix_post_g[layer])
        h = rmsnorm(x, ffn_pre_g[layer]) * (1.0 + scale_f) + shift_f
        y = conv_ffn(h, ffn_w_up[layer], ffn_conv_w[layer], ffn_conv_b[layer], ffn_w_down[layer])
        x = x + gate_f * rmsnorm(y, ffn_post_g[layer])
    return x
```

```python
import math
import numpy as np
import ml_dtypes
import concourse.bass as bass
import concourse.mybir as mybir
from concourse.bass_utils import run_bass_kernel_spmd

F32 = mybir.dt.float32
BF16 = mybir.dt.bfloat16
AF = mybir.ActivationFunctionType
ALU = mybir.AluOpType
AX = mybir.AxisListType
NPBF = ml_dtypes.bfloat16

D = 1024
S = 4096
DFF = 2816
NCH = 22
NT = 17
EPS = 1e-6
NEG = -30000.0
COMPUTE = ("pe", "act", "dve", "pool")


class Op:
    __slots__ = ("eng", "fn", "deps", "sig", "ticket", "kind", "sem", "semval", "idx", "prev_same_sem")


class Prog:
    def __init__(self, nc, same_eng_sync=True):
        self.nc = nc
        self.ops = []
        self.lastw = {}
        self.readers = {}
        self.same_eng_sync = same_eng_sync
        self.ndma = {"sp": 16, "act": 6, "pool": 8}
        self.dma_count = {"sp": 0, "act": 0, "pool": 0}
        self.cc_count = 0
        self._uid = 0
        self.fence_deps = []
        self.stats = {}
        self.fuse_waits = True

    def fence(self):
        last = {}
        for op in self.ops:
            if op.kind == "c":
                last[("c", op.eng)] = op.idx
            else:
                last[op.sem] = op.idx
        self.fence_deps = list(last.values())

    def uid(self, p="t"):
        self._uid += 1
        return f"{p}{self._uid}"

    def add(self, eng, fn, r=(), w=(), kind="c"):
        op = Op()
        op.eng, op.fn, op.kind, op.idx = eng, fn, kind, len(self.ops)
        op.sig = kind != "c"
        op.ticket = None
        deps = set()
        for k in r:
            d = self.lastw.get(k)
            if d is not None:
                deps.add(d)
        for k in w:
            d = self.lastw.get(k)
            if d is not None:
                deps.add(d)
            rd = self.readers.get(k)
            if rd:
                for e, lst in rd.items():
                    deps.update(lst)
        for k in r:
            rd = self.readers.setdefault(k, {})
            if kind == "c":
                rd[eng] = [op.idx]
            else:
                rd.setdefault("dma", []).append(op.idx)
        for k in w:
            self.lastw[k] = op.idx
            self.readers[k] = {}
        deps.update(self.fence_deps)
        deps.discard(op.idx)
        best = {}
        keep = []
        for d in deps:
            o = self.ops[d]
            if o.kind == "c":
                if o.eng not in best or best[o.eng] < d:
                    best[o.eng] = d
            else:
                keep.append(d)
        op.deps = keep + list(best.values())
        if kind == "dma":
            n = self.ndma[eng]
            j = self.dma_count[eng]
            self.dma_count[eng] += 1
            op.sem = (eng, j % n)
            op.semval = 16 * (j // n + 1)
        elif kind == "cc":
            self.cc_count += 1
            op.sem = ("cc", 0)
            op.semval = self.cc_count
        self.ops.append(op)
        return op.idx

    def emit(self):
        nc = self.nc
        ops = self.ops
        for op in ops:
            for d in op.deps:
                o = ops[d]
                if o.kind == "c":
                    if o.eng == op.eng and op.kind == "c" and (o.eng == "pe" or not self.same_eng_sync):
                        continue
                    o.sig = True
        cnt = {e: 0 for e in COMPUTE}
        for op in ops:
            if op.kind == "c" and op.sig:
                cnt[op.eng] += 1
                op.ticket = cnt[op.eng]
        names = [("c", e) for e in COMPUTE] + [("cc", 0)]
        for q, n in self.ndma.items():
            names += [(q, i) for i in range(n)]
        import contextlib
        with contextlib.ExitStack() as st:
            sems = {k: st.enter_context(nc.semaphore(f"s_{k[0]}_{k[1]}")) for k in names}
            block = st.enter_context(nc.Block())
            streams = {e: [o for o in ops if o.eng == e] for e in ("pe", "act", "dve", "pool", "sp")}

            def run(ename, e):
                waited = {}

                def wait(key, val):
                    if waited.get(key, 0) >= val:
                        return
                    waited[key] = val
                    self.stats[(ename, "wait")] = self.stats.get((ename, "wait"), 0) + 1
                    e.wait_ge(sems[key], val)

                class Rec:
                    def __init__(self):
                        self.first = None

                    def __getattr__(self, name):
                        attr = getattr(e, name)
                        if not callable(attr):
                            return attr

                        def call(*a, **k):
                            r = attr(*a, **k)
                            if self.first is None:
                                self.first = r
                            return r
                        return call

                for op in streams[ename]:
                    need = []
                    for d in sorted(op.deps):
                        o = ops[d]
                        if o.kind == "c":
                            if o.eng == ename and op.kind == "c" and (ename == "pe" or not self.same_eng_sync):
                                continue
                            need.append((("c", o.eng), o.ticket))
                        else:
                            need.append((o.sem, o.semval))
                    if op.kind == "dma" and op.semval > 16:
                        need.append((op.sem, op.semval - 16))
                    mx = {}
                    for k, v in need:
                        if waited.get(k, 0) < v and mx.get(k, 0) < v:
                            mx[k] = v
                    items = list(mx.items())
                    fused = items.pop() if (items and self.fuse_waits) else None
                    for k, v in items:
                        wait(k, v)
                    rec = Rec()
                    ins = op.fn(rec)
                    if fused is not None:
                        k, v = fused
                        waited[k] = v
                        self.stats[(ename, "fwait")] = self.stats.get((ename, "fwait"), 0) + 1
                        rec.first._wait_ge(sems[k], v)
                    self.stats[(ename, op.kind)] = self.stats.get((ename, op.kind), 0) + 1
                    if op.kind == "c":
                        if op.sig:
                            ins.then_inc(sems[("c", ename)], 1)
                    elif op.kind == "dma":
                        ins.then_inc(sems[op.sem], 16)
                    else:
                        ins.then_inc(sems[op.sem], 1)
                last = {}
                for op in streams[ename]:
                    if op.kind != "c":
                        last[op.sem] = max(last.get(op.sem, 0), op.semval)
                for k, v in last.items():
                    wait(k, v)

            block.tensor(lambda e: run("pe", e))
            block.scalar(lambda e: run("act", e))
            block.vector(lambda e: run("dve", e))
            block.gpsimd(lambda e: run("pool", e))
            block.sync(lambda e: run("sp", e))


_DTSIZE = {F32: 4, BF16: 2}
_ARENA = [None]


class Arena:
    def __init__(self, nc, nbytes=210944):
        self.t = nc.alloc_sbuf_tensor("arena", [128, nbytes], mybir.dt.uint8)
        self.off = 0
        self.size = nbytes
        self.peak = 0

    def alloc(self, shape, dtype):
        n = 1
        for v in shape[1:]:
            n *= v
        nb = (n * _DTSIZE[dtype] + 31) // 32 * 32
        assert self.off + nb <= self.size, f"SBUF arena overflow: need {nb} at {self.off}"
        v = self.t[0:shape[0], self.off:self.off + n * _DTSIZE[dtype]].bitcast(dtype)
        self.off += nb
        self.peak = max(self.peak, self.off)
        if len(shape) == 3:
            v = v.rearrange("p (a b) -> p a b", a=shape[1])
        elif len(shape) == 4:
            v = v.rearrange("p (a b c) -> p a b c", a=shape[1], b=shape[2])
        return v


def salloc(nc, name, shape, dtype):
    return _ARENA[0].alloc(list(shape), dtype)


class Rot:
    def __init__(self, nc, name, n, shape, dtype):
        self.tiles = [salloc(nc, f"{name}_{i}", shape, dtype) for i in range(n)]
        self.name = name
        self.i = 0

    def next(self):
        j = self.i % len(self.tiles)
        self.i += 1
        return self.tiles[j], (self.name, j)


def dma(P, q, out, in_, r, w, **kw):
    return P.add(q, lambda e: e.dma_start(out=out, in_=in_, **kw), r=r, w=w, kind="dma")


class Ctx:
    pass


def scope_open(C):
    import copy
    Cc = copy.copy(C)
    Cc._mark = C.A.off
    return Cc


def scope_close(Cc):
    Cc.A.off = Cc._mark
    Cc.P.fence()


def setup_common(nc, P):
    C = Ctx()
    C.nc, C.P = nc, P
    C.A = Arena(nc)
    _ARENA[0] = C.A
    C.ps = [nc.alloc_psum_tensor(f"ps{i}", [128, 512], F32) for i in range(8)]
    C.psk = [("ps", i) for i in range(8)]
    C.ident_f_d = nc.dram_tensor("ident_f", [128, 128], F32, kind="ExternalInput").ap()
    C.ident_f = salloc(nc, "ident_f_sb", [128, 128], F32)
    C.ident_b = salloc(nc, "ident_b_sb", [128, 128], BF16)
    dma(P, "sp", C.ident_f[:], C.ident_f_d, r=[], w=["ident_f"])
    C.eps_col = salloc(nc, "eps_col", [128, 1], F32)
    C.ones_f = salloc(nc, "ones_f", [128, 128], F32)
    P.add("pool", lambda e: e.memset(C.ones_f[:], 1.0), r=[], w=["ones_f"])
    C.diag_rot = Rot(nc, "diag", 2, [128, 128], F32)
    C.one_col = salloc(nc, "one_col", [128, 1], F32)
    P.add("pool", lambda e: e.memset(C.one_col[:], 1.0), r=[], w=["one_col"])
    P.add("pool", lambda e: e.memset(C.eps_col[:], EPS), r=[], w=["eps"])
    P.add("dve", lambda e: e.tensor_copy(out=C.ident_b[:], in_=C.ident_f[:]), r=["ident_f"], w=["ident_b"])
    return C


def emit_rstd(C, ss_ap, ss_key, n_feat, rot):
    P = C.P
    t, k = rot.next()
    shp = ss_ap.shape
    o = t[0:shp[0], 0:shp[1]]
    P.add("act", lambda e: e.activation(out=o, in_=ss_ap, func=AF.Ln, bias=C.eps_col[0:shp[0], :], scale=1.0 / n_feat),
          r=[ss_key, "eps"], w=[k])
    P.add("act", lambda e: e.activation(out=o, in_=o, func=AF.Exp, scale=-0.5), r=[k], w=[k])
    return o, k


def setup_cond(C, c_col_d):
    nc, P = C.nc, C.P
    C.cond = salloc(nc, "cond", [128, 8], F32)
    ccol = salloc(nc, "ccol", [128, 8], F32)
    tmp = salloc(nc, "cond_tmp", [128, 8], F32)
    dma(P, "sp", ccol[:], c_col_d, r=[], w=["ccol"])
    P.add("act", lambda e: e.activation(out=tmp[:], in_=ccol[:], func=AF.Exp, scale=-1.0), r=["ccol"], w=["cond_tmp"])
    P.add("dve", lambda e: e.tensor_scalar(out=tmp[:], in0=tmp[:], scalar1=1.0, scalar2=None, op0=ALU.add), r=["cond_tmp"], w=["cond_tmp"])
    P.add("dve", lambda e: e.reciprocal(out=tmp[:], in_=tmp[:]), r=["cond_tmp"], w=["cond_tmp"])
    P.add("dve", lambda e: e.tensor_tensor(out=C.cond[:], in0=ccol[:], in1=tmp[:], op=ALU.mult), r=["cond_tmp", "ccol"], w=["cond"])


def emit_mod(C, c_col_d, ada_w_t_d, ada_b_col_d, name, mod=None):
    nc, P = C.nc, C.P
    if not hasattr(C, "cond"):
        setup_cond(C, c_col_d)
    if not hasattr(C, "adaw_rot"):
        C.adaw_rot = Rot(nc, "adaw", 2, [128, 8, 128], F32)
    if mod is None:
        mod = salloc(nc, f"mod_{name}", [128, 48], F32)
    bcol = salloc(nc, f"adab_{name}", [128, 48], F32)
    dma(P, "sp", bcol[:], ada_b_col_d, r=[], w=[f"adab_{name}"])
    ps, psk = C.ps[7], C.psk[7]
    for c in range(48):
        wt, wk = C.adaw_rot.next()
        dma(P, "act", wt[:], ada_w_t_d[c], r=[], w=[wk])

        def f(e, wt=wt, c=c):
            ins = None
            for k in range(8):
                ins = e.matmul(ps[:, c:c + 1], lhsT=wt[:, k, :], rhs=C.cond[:, k:k + 1], start=(k == 0), stop=(k == 7))
            return ins
        P.add("pe", f, r=[wk, "cond"], w=[psk])
    P.add("dve", lambda e: e.tensor_tensor(out=mod[:], in0=ps[:, 0:48], in1=bcol[:], op=ALU.add),
          r=[psk, f"adab_{name}"], w=[f"mod_{name}"])
    return mod, f"mod_{name}"


def emit_bcast_row(C, col_ap, col_key, out_tile, out_key):
    nc, P = C.nc, C.P
    for half in range(2):
        ps, psk = C.ps[6 + half], C.psk[6 + half]
        for kk in range(4):
            k = half * 4 + kk
            dg, dk = C.diag_rot.next()
            P.add("dve", lambda e, dg=dg, k=k: e.tensor_scalar(out=dg[:], in0=C.ident_f[:], scalar1=col_ap[:, k:k + 1], scalar2=None, op0=ALU.mult),
                  r=["ident_f", col_key], w=[dk])
            P.add("pe", lambda e, dg=dg, kk=kk, ps=ps: e.matmul(ps[:, kk * 128:(kk + 1) * 128], lhsT=C.ones_f[:], rhs=dg[:], start=True, stop=True),
                  r=["ones_f", dk], w=[psk])
        P.add("act", lambda e, ps=ps, half=half: e.copy(out=out_tile[:, half * 512:(half + 1) * 512], in_=ps[:]), r=[psk], w=[out_key])


def emit_norm_T(C, x_ap, x_key, a_col, b_col, ab_key, hT_ap, hT_key):
    nc, P = C.nc, C.P
    if not hasattr(C, "n_junk"):
        C.n_junk = Rot(nc, "njunk", 1, [128, 1024], BF16)
        C.n_ss = Rot(nc, "nss", 4, [128, 1], F32)
        C.n_rs = Rot(nc, "nrs", 4, [128, 1], F32)
        C.n_xn = Rot(nc, "nxn", 2, [128, 1024], BF16)
    jt, jk = C.n_junk.next()
    ss, ssk = C.n_ss.next()
    P.add("act", lambda e: e.activation(out=jt[:], in_=x_ap, func=AF.Square, accum_out=ss[:]), r=[x_key], w=[jk, ssk])
    rs, rsk = emit_rstd(C, ss[:], ssk, D, C.n_rs)
    xn, xnk = C.n_xn.next()
    P.add("act", lambda e: e.activation(out=xn[:], in_=x_ap, func=AF.Copy, scale=rs), r=[x_key, rsk], w=[xnk])
    ps, psk = C.ps[6], C.psk[6]
    psb = ps[:].bitcast(BF16)

    def ft(e):
        ins = None
        for k in range(8):
            ins = e.transpose(psb[:, k * 128:(k + 1) * 128], xn[:, k * 128:(k + 1) * 128], C.ident_b[:])
        return ins
    P.add("pe", ft, r=[xnk, "ident_b"], w=[psk])
    for k in range(8):
        P.add("dve", lambda e, k=k: e.tensor_scalar(out=hT_ap[:, k, :], in0=psb[:, k * 128:(k + 1) * 128],
                                                     scalar1=a_col[:, k:k + 1], scalar2=b_col[:, k:k + 1], op0=ALU.mult, op1=ALU.add),
              r=[psk, ab_key], w=[hT_key])


def emit_resid_update(C, y_banks, y_keys, x_ap, x_key, gm_tile, gm_key):
    nc, P = C.nc, C.P
    if not hasattr(C, "r_junk"):
        C.r_junk = Rot(nc, "rjunk", 2, [128, 512], BF16)
        C.r_ss = Rot(nc, "rss", 4, [128, 2], F32)
        C.r_s1 = Rot(nc, "rs1", 4, [128, 1], F32)
        C.r_rs = Rot(nc, "rrs", 4, [128, 1], F32)
        C.r_tmp = Rot(nc, "rtmp", 1, [128, 1024], F32)
    ss, ssk = C.r_ss.next()
    for h in range(2):
        jt, jk = C.r_junk.next()
        P.add("act", lambda e, h=h, jt=jt: e.activation(out=jt[:], in_=y_banks[h][:], func=AF.Square, accum_out=ss[:, h:h + 1]),
              r=[y_keys[h]], w=[jk, (ssk, h)])
    s1, s1k = C.r_s1.next()
    P.add("dve", lambda e: e.tensor_tensor(out=s1[:], in0=ss[:, 0:1], in1=ss[:, 1:2], op=ALU.add), r=[(ssk, 0), (ssk, 1)], w=[s1k])
    rs, rsk = emit_rstd(C, s1[:], s1k, D, C.r_rs)
    tmp, tk = C.r_tmp.next()
    for h in range(2):
        P.add("dve", lambda e, h=h: e.scalar_tensor_tensor(out=tmp[:, h * 512:(h + 1) * 512], in0=y_banks[h][:], scalar=rs,
                                                            in1=gm_tile[:, h * 512:(h + 1) * 512], op0=ALU.mult, op1=ALU.mult),
              r=[y_keys[h], rsk, gm_key], w=[(tk, h)])
    P.add("pool", lambda e: e.tensor_tensor(out=x_ap, in0=x_ap, in1=tmp[:], op=ALU.add), r=[(tk, 0), (tk, 1), x_key], w=[x_key])


GROUPS = [(0, 1), (1, 4), (5, 4), (9, 4), (13, 4)]


def tok_setup_post(C, T, flags):
    nc, P = C.nc, C.P
    mod, modk = T["mod"], T["modk"]
    gcol = salloc(nc, P.uid("gcol"), [128, 3, 8], F32)
    gck = P.uid("gck")
    dma(P, "sp", gcol[:], T["gains_post"], r=[], w=[gck])
    small = salloc(nc, P.uid("small"), [128, 3, 8], F32)
    smk = P.uid("smk")
    P.add("dve", lambda e: e.tensor_tensor(out=small[:, 0, :], in0=mod[:, 16:24], in1=gcol[:, 0, :], op=ALU.mult), r=[modk, gck], w=[(smk, 0)])
    P.add("dve", lambda e: e.scalar_tensor_tensor(out=small[:, 1, :], in0=mod[:, 32:40], scalar=1.0, in1=gcol[:, 1, :], op0=ALU.add, op1=ALU.mult),
          r=[modk, gck], w=[(smk, 1)])
    P.add("dve", lambda e: e.tensor_tensor(out=small[:, 2, :], in0=mod[:, 40:48], in1=gcol[:, 2, :], op=ALU.mult), r=[modk, gck], w=[(smk, 2)])
    if not hasattr(C, "wout_b"):
        C.gm = salloc(nc, "gm", [128, 1024], F32)
        C.gf = salloc(nc, "gf", [128, 1024], F32)
        C.wout_b = salloc(nc, "wout_b", [128, 8, 1024], BF16)
        C.wdn_b = salloc(nc, "wdn_b", [128, NCH, 1024], BF16)
        C.wstage = Rot(nc, "wstage", 2, [128, 1024], F32)
        C.wup_b = Rot(nc, "wup_b", 2, [128, 8, 256], BF16)
        C.convc = salloc(nc, "convc", [128, 44, 4], F32)
        C.tails = salloc(nc, "tails", [128, 44, 2], F32)
        C.oa = Rot(nc, "oa", 2, [128, 8, 128], BF16)
        C.ob = Rot(nc, "ob", 2, [128, 8, 128], BF16)
        C.osel = Rot(nc, "osel", 2, [128, 8, 128], BF16)
        C.U = Rot(nc, "U", 2, [128, 514], F32)
        C.cv = Rot(nc, "cv", 4, [128, 512], F32)
        C.gl = Rot(nc, "gl", 1, [128, 512], F32)
        C.gT = salloc(nc, "gT", [128, NCH, 512], BF16)
    gmk, gfk = P.uid("gmk"), P.uid("gfk")
    emit_bcast_row(C, small[:, 0, :], (smk, 0), C.gm, "gm")
    emit_bcast_row(C, small[:, 2, :], (smk, 2), C.gf, "gf")
    for k in range(8):
        st, sk = C.wstage.next()
        dma(P, "sp", st[:], T["w_out_t"][:, k, :], r=[], w=[sk])
        P.add("act", lambda e, st=st, k=k: e.copy(out=C.wout_b[:, k, :], in_=st[:]), r=[sk], w=[("wout_b", k)])

    def load_w_down(C=C, T=T):
        for k in range(NCH):
            st, sk = C.wstage.next()
            dma(P, "sp", st[:], T["w_down_t"][:, k, :], r=[], w=[sk])
            P.add("pool", lambda e, st=st, k=k: e.tensor_copy(out=C.wdn_b[:, k, :], in_=st[:]), r=[sk], w=[("wdn_b", k)])
    T["load_w_down"] = load_w_down
    dma(P, "sp", C.convc[:], T["conv_col"], r=[], w=["convc"])
    P.add("pool", lambda e: e.memset(C.tails[:], 0.0), r=[], w=[("tails", c) for c in range(44)])
    T["small"], T["smk"] = small, smk


def tok_setup_pre(C, T):
    nc, P = C.nc, C.P
    mod, modk = T["mod"], T["modk"]
    gcol = salloc(nc, P.uid("gpre"), [128, 8], F32)
    gck = P.uid("gprek")
    dma(P, "sp", gcol[:], T["gains_pre"], r=[], w=[gck])
    am = salloc(nc, P.uid("am"), [128, 8], F32)
    amk = P.uid("amk")
    P.add("dve", lambda e: e.scalar_tensor_tensor(out=am[:], in0=mod[:, 8:16], scalar=1.0, in1=gcol[:], op0=ALU.add, op1=ALU.mult),
          r=[modk, gck], w=[amk])
    T["am"], T["amk"] = am, amk


def pipeline(stages, n):
    ns = len(stages)
    for step in range(n + ns - 1):
        for si in range(ns):
            i = step - si
            if 0 <= i < n:
                stages[si](i)


def emit_tok(C, Tp, Tq, x_src_d, x_dst_d, oT_all_d, hT_src_d, flags, pre_setup=None):
    nc, P = C.nc, C.P
    if not hasattr(C, "xg"):
        C.xg = Rot(nc, "xg", 2, [128, 4, D], F32)
        C.hT = Rot(nc, "hTf", 2, [128, 8, 512], BF16)
    woutk = [("wout_b", k) for k in range(8)]
    wdnk = [("wdn_b", k) for k in range(NCH)]
    ybank = [0]

    def next_y():
        yb = [C.ps[ybank[0]], C.ps[ybank[0] + 1]]
        ybk = [C.psk[ybank[0]], C.psk[ybank[0] + 1]]
        ybank[0] = (ybank[0] + 2) % 4
        return yb, ybk

    if Tp is None and pre_setup is not None:
        pre_setup()
        pre_setup = None
    for gi, (t0, ntl) in enumerate(GROUPS):
        N = ntl * 128
        if Tp is None and gi == 0:
            continue
        xg, xgk = C.xg.next()
        for ti in range(ntl):
            tt = t0 + ti
            dma(P, "sp", xg[:, ti, :], x_src_d[tt * 128:(tt + 1) * 128, :], r=["x_src"], w=[(xgk, ti)])
        if Tp is not None:
            small, smk, mod, modk = Tp["small"], Tp["smk"], Tp["mod"], Tp["modk"]
            hT, hTk = C.hT.next()
            ys = {}

            def stA(ti, gi=gi, t0=t0):
                tt = t0 + ti
                oa, oak = C.oa.next()
                ob, obk = C.ob.next()
                osel, osk = C.osel.next()
                cola = 0 if gi == 0 else (tt - 1) * 128
                colb = 2048 + (tt - 1) * 128
                for r in range(2):
                    for hp in range(2):
                        kk = 4 * r + 2 * hp
                        dma(P, "sp", oa[:, kk:kk + 2, :], oT_all_d[hp][r, :, cola:cola + 128].rearrange("(k p) n -> p k n", p=128), r=["oT_all"], w=[(oak, r, hp)])
                        dma(P, "act", ob[:, kk:kk + 2, :], oT_all_d[hp][r, :, colb:colb + 128].rearrange("(k p) n -> p k n", p=128), r=["oT_all"], w=[(obk, r, hp)])
                P.add("dve", lambda e: e.tensor_scalar(out=osel[:], in0=oa[:], scalar1=flags[:, 0:1], scalar2=None, op0=ALU.mult),
                      r=[(oak, r, hp) for r in range(2) for hp in range(2)] + ["flags"], w=[osk])
                P.add("dve", lambda e: e.scalar_tensor_tensor(out=osel[:], in0=ob[:], scalar=flags[:, 1:2], in1=osel[:], op0=ALU.mult, op1=ALU.add),
                      r=[(obk, r, hp) for r in range(2) for hp in range(2)] + ["flags", osk], w=[osk])
                yb, ybk = next_y()
                for h in range(2):
                    def f(e, h=h):
                        ins = None
                        for k in range(8):
                            ins = e.matmul(yb[h][:], lhsT=osel[:, k, :], rhs=C.wout_b[:, k, h * 512:(h + 1) * 512], start=(k == 0), stop=(k == 7))
                        return ins
                    P.add("pe", f, r=[osk] + woutk, w=[ybk[h]])
                ys[ti] = (yb, ybk)

            def stB(ti, xg=xg, xgk=xgk):
                yb, ybk = ys[ti]
                emit_resid_update(C, yb, ybk, xg[:, ti, :], (xgk, ti), C.gm, "gm")

            def stC(ti, xg=xg, xgk=xgk, hT=hT, hTk=hTk):
                emit_norm_T(C, xg[:, ti, :], (xgk, ti), small[:, 1, :], mod[:, 24:32], (smk, 1), hT[:, :, ti * 128:(ti + 1) * 128], (hTk, ti))
            pipeline([stA, stB, stC], ntl)
            if Tp.get("load_w_down") is not None:
                Tp.pop("load_w_down")()
            hTkeys = [(hTk, ti) for ti in range(ntl)]
            wus, cvss = {}, {}

            def stW(j, gi=gi):
                wu, wuk = C.wup_b.next()
                wc = Tp["wcache"]
                if gi == 0:
                    for half in range(2):
                        st, sk = C.wstage.next()
                        dma(P, "sp", st[:], Tp["w_up_t"][j, half].rearrange("p k n -> p (k n)"), r=[], w=[sk])
                        if half == 0:
                            P.add("dve", lambda e, st=st, half=half: e.tensor_copy(out=wu[:, :, half * 128:(half + 1) * 128], in_=st[:].rearrange("p (k n) -> p k n", k=8)),
                                  r=[sk], w=[(wuk, half)])
                        else:
                            P.add("act", lambda e, st=st, half=half: e.copy(out=wu[:, :, half * 128:(half + 1) * 128], in_=st[:].rearrange("p (k n) -> p k n", k=8)),
                                  r=[sk], w=[(wuk, half)])
                    dma(P, "sp", wc[j], wu[:].rearrange("p k n -> p (k n)"), r=[(wuk, 0), (wuk, 1)], w=[("wcache", j)])
                else:
                    dma(P, "sp", wu[:].rearrange("p k n -> p (k n)"), wc[j], r=[("wcache", j)], w=[(wuk, 0), (wuk, 1)])
                wus[j] = (wu, wuk)

            def stX(j, hT=hT, N=N, gi=gi):
                wu, wuk = wus.pop(j)
                cvs = []
                for half in range(2):
                    c = j + 22 * half
                    pb = 4 + half + 2 * (j % 2)
                    ps, psk = C.ps[pb], C.psk[pb]

                    def f(e, half=half, ps=ps):
                        ins = None
                        for k in range(8):
                            ins = e.matmul(ps[:, 0:N], lhsT=wu[:, k, half * 128:(half + 1) * 128], rhs=hT[:, k, 0:N], start=(k == 0), stop=(k == 7))
                        return ins
                    P.add("pe", f, r=[(wuk, half)] + hTkeys, w=[psk])
                    U, Uk = C.U.next()
                    P.add("act", lambda e, U=U, ps=ps: e.copy(out=U[:, 2:2 + N], in_=ps[:, 0:N]), r=[psk], w=[Uk])
                    P.add("pool", lambda e, U=U, c=c: e.tensor_copy(out=U[:, 0:2], in_=C.tails[:, c, :]), r=[("tails", c)], w=[(Uk, "t")])
                    cv, cvk = C.cv.next()
                    P.add("act", lambda e, cv=cv, ps=ps, c=c: e.activation(out=cv[:, 0:N], in_=ps[:, 0:N], func=AF.Identity,
                                                                          scale=C.convc[:, c, 2:3], bias=C.convc[:, c, 3:4]),
                          r=[psk, "convc"], w=[cvk])
                    P.add("dve", lambda e, cv=cv, U=U, c=c: e.scalar_tensor_tensor(out=cv[:, 0:N], in0=U[:, 1:1 + N], scalar=C.convc[:, c, 1:2], in1=cv[:, 0:N],
                                                                                  op0=ALU.mult, op1=ALU.add),
                          r=[Uk, (Uk, "t"), cvk, "convc"], w=[cvk])
                    P.add("dve", lambda e, cv=cv, U=U, c=c: e.scalar_tensor_tensor(out=cv[:, 0:N], in0=U[:, 0:N], scalar=C.convc[:, c, 0:1], in1=cv[:, 0:N],
                                                                                  op0=ALU.mult, op1=ALU.add),
                          r=[Uk, (Uk, "t"), cvk, "convc"], w=[cvk])
                    if gi == 0:
                        P.add("pool", lambda e, U=U, c=c: e.tensor_scalar(out=C.tails[:, c, :], in0=U[:, N:N + 2], scalar1=flags[:, 2:3], scalar2=None, op0=ALU.mult),
                              r=[Uk, "flags"], w=[("tails", c)])
                    else:
                        P.add("pool", lambda e, U=U, c=c: e.tensor_copy(out=C.tails[:, c, :], in_=U[:, N:N + 2]), r=[Uk], w=[("tails", c)])
                    cvs.append((cv, cvk))
                cvss[j] = cvs

            def stY(j, N=N):
                gl, glk = C.gl.next()
                (cg, cgk), (cvv, cvvk) = cvss.pop(j)
                P.add("act", lambda e: e.activation(out=gl[:, 0:N], in_=cg[:, 0:N], func=AF.Gelu_apprx_tanh), r=[cgk], w=[glk])
                P.add("dve", lambda e: e.tensor_tensor(out=C.gT[:, j, 0:N], in0=gl[:, 0:N], in1=cvv[:, 0:N], op=ALU.mult),
                      r=[glk, cvvk], w=[("gT", j)])
            pipeline([stW, stX, stY], NCH)
            gTk = [("gT", j) for j in range(NCH)]
            if pre_setup is not None:
                pre_setup()
                pre_setup = None
            ys2 = {}

            def stD(ti):
                yb, ybk = next_y()
                for h in range(2):
                    def f(e, h=h):
                        ins = None
                        for j in range(NCH):
                            ins = e.matmul(yb[h][:], lhsT=C.gT[:, j, ti * 128:(ti + 1) * 128], rhs=C.wdn_b[:, j, h * 512:(h + 1) * 512],
                                           start=(j == 0), stop=(j == NCH - 1))
                        return ins
                    P.add("pe", f, r=gTk + wdnk, w=[ybk[h]])
                ys2[ti] = (yb, ybk)

            def stE(ti, xg=xg, xgk=xgk):
                yb, ybk = ys2[ti]
                emit_resid_update(C, yb, ybk, xg[:, ti, :], (xgk, ti), C.gf, "gf")
            stages = [stD, stE]
            hT2 = None
            if Tq is not None and gi > 0:
                hT2, hT2k = C.hT.next()

                def stF(ti, xg=xg, xgk=xgk, hT2=hT2, hT2k=hT2k):
                    emit_norm_T(C, xg[:, ti, :], (xgk, ti), Tq["am"], Tq["mod"][:, 0:8], Tq["amk"], hT2[:, :, ti * 128:(ti + 1) * 128], (hT2k, ti))
                stages.append(stF)
            pipeline(stages, ntl)
            if hT2 is not None:
                g = gi - 1
                dma(P, "sp", hT_src_d[:, g * 512:(g + 1) * 512].rearrange("(k p) n -> p k n", p=128), hT2[:],
                    r=[(hT2k, ti) for ti in range(4)], w=["hT_src"])
        elif Tq is not None and gi > 0:
            hT, hTk = C.hT.next()
            for ti in range(ntl):
                emit_norm_T(C, xg[:, ti, :], (xgk, ti), Tq["am"], Tq["mod"][:, 0:8], Tq["amk"], hT[:, :, ti * 128:(ti + 1) * 128], (hTk, ti))
            g = gi - 1
            dma(P, "sp", hT_src_d[:, g * 512:(g + 1) * 512].rearrange("(k p) n -> p k n", p=128), hT[:],
                r=[(hTk, ti) for ti in range(4)], w=["hT_src"])
        if Tp is not None:
            for ti in range(ntl):
                tt = t0 + ti
                dma(P, "sp", x_dst_d[tt * 128:(tt + 1) * 128, :], xg[:, ti, :], r=[(xgk, ti)], w=["x_dst"])


def build_tok(do_post, do_pre):
    nc = bass.Bass("TRN2", target_bir_lowering=False)
    P = Prog(nc)
    C = setup_common(nc, P)
    din = lambda name, shape, dt=F32: nc.dram_tensor(name, shape, dt, kind="ExternalInput").ap()
    x_in = din("x_in", [NT * 128, D])
    flags_d = din("flags", [128, 4])
    c_col = din("c_col", [128, 8])
    flags = salloc(nc, "flags_sb", [128, 4], F32)
    dma(P, "sp", flags[:], flags_d, r=[], w=["flags"])
    Tp = Tq = None
    oT_all = hT_src = x_out = None
    if do_post:
        Tp = {"w_out_t": din("w_out_t", [128, 8, 1024]), "w_down_t": din("w_down_t", [128, NCH, 1024]),
              "w_up_t": din("w_up_t", [NCH, 2, 128, 8, 128]), "conv_col": din("conv_col", [128, 44, 4]),
              "gains_post": din("gains_post", [128, 3, 8]), "wcache": nc.dram_tensor("wcache", [NCH, 128, 8 * 256], BF16).ap()}
        oT_all = [din("oT_allA", [2, 256, S], BF16), din("oT_allB", [2, 256, S], BF16)]
        Tp["mod"], Tp["modk"] = emit_mod(C, c_col, din("ada_w_t_post", [48, 128, 8, 128]), din("ada_b_col_post", [128, 48]), "post")
        tok_setup_post(C, Tp, flags)
        x_out = nc.dram_tensor("x_out", [NT * 128, D], F32, kind="ExternalOutput").ap()
    if do_pre:
        Tq = {"gains_pre": din("gains_pre", [128, 8])}
        Tq["mod"], Tq["modk"] = emit_mod(C, c_col, din("ada_w_t_pre", [48, 128, 8, 128]), din("ada_b_col_pre", [128, 48]), "pre")
        tok_setup_pre(C, Tq)
        hT_src = nc.dram_tensor("hT_src", [D, 2048], BF16, kind="ExternalOutput").ap()
    emit_tok(C, Tp, Tq, x_in, x_out, oT_all, hT_src, flags)
    print("tok SBUF peak", C.A.peak)
    P.emit()
    return nc


def col_layout(v):
    return np.ascontiguousarray(np.asarray(v, np.float32).reshape(-1, 128).T)


def ada_w_tiles(w):
    return np.ascontiguousarray(np.asarray(w, np.float32).reshape(8, 128, 48, 128).transpose(2, 1, 0, 3))


def k_tiles(w):
    w = np.asarray(w, np.float32)
    return np.ascontiguousarray(w.reshape(w.shape[0] // 128, 128, w.shape[1]).transpose(1, 0, 2))


def w_up_tiles(w):
    w = np.asarray(w, np.float32)
    g = w[:, :DFF].reshape(8, 128, NCH, 128)
    v = w[:, DFF:].reshape(8, 128, NCH, 128)
    return np.ascontiguousarray(np.stack([g, v], axis=0).transpose(3, 0, 2, 1, 4))


def conv_cols(cw, cb):
    a = np.concatenate([np.asarray(cw, np.float32), np.asarray(cb, np.float32)[None]], 0)
    return np.ascontiguousarray(a.reshape(4, 44, 128).transpose(2, 1, 0))


def core_flags(core):
    half = core % 2
    f = np.zeros((128, 4), np.float32)
    f[:, 0] = 1.0 - half
    f[:, 1] = float(half)
    f[:, 2] = float(half)
    return f


TW = 4480
GW = 4608


def t5_bucket_np(d):
    d = np.maximum(d, 0)
    df = np.maximum(d, 16).astype(np.float32)
    large = 16 + (np.log(df / np.float32(16)) / np.float32(math.log(128.0)) * np.float32(16)).astype(np.int32)
    large = np.minimum(large, 31)
    return np.where(d < 16, d, large)


def onehot_tables():
    n = np.arange(GW)
    d = n - 511
    b = t5_bucket_np(d)
    oh = np.zeros((2, 33, GW), np.float32)
    va = d >= 0
    oh[0, b[va], n[va]] = 1.0
    oh[0, 32, ~va] = NEG
    cnt = ((d >= 0) & (d <= 128)).astype(np.int64) + ((d >= 0) & (d <= 512) & (d % 4 == 0)) + ((d >= 0) & (d <= 2048) & (d % 16 == 0))
    vb = cnt > 0
    oh[1, b[vb], n[vb]] = 1.0
    oh[1, 32, vb] = np.log(cnt[vb].astype(np.float64)).astype(np.float32)
    oh[1, 32, ~vb] = NEG
    return oh


def causal_masks():
    i = np.arange(128)[:, None]
    c = np.arange(128)[None, :]
    m = np.zeros((2, 128, 128), np.float32)
    m[0][c < i] = NEG
    m[1][c <= i] = NEG
    return m


def emit_toeplitz_build(C, g2_t, slot, rb_col, rb_key, oh_d):
    nc, P = C.nc, C.P
    if not hasattr(C, "tz_oh"):
        C.tz_oh = Rot(nc, "tz_oh", 2, [33, 512], F32)
        C.tz_rb = Rot(nc, "tz_rb", 2, [33, 128], F32)
        C.tz_g = Rot(nc, "tz_g", 2, [128, 512], F32)
    rb, rbk = C.tz_rb.next()
    P.add("dve", lambda e: e.tensor_scalar(out=rb[:], in0=C.ones_f[0:33, :], scalar1=rb_col, scalar2=None, op0=ALU.mult), r=["ones_f", rb_key], w=[rbk])
    g2k = ("g2", slot)
    for cc in range(GW // 512):
        oh, ohk = C.tz_oh.next()
        dma(P, "sp", oh[:], oh_d[:, cc * 512:(cc + 1) * 512], r=[], w=[ohk])
        ps, psk = C.ps[6], C.psk[6]
        P.add("pe", lambda e, oh=oh, ps=ps: e.matmul(ps[:], lhsT=rb[:], rhs=oh[:], start=True, stop=True), r=[rbk, ohk], w=[psk])
        g, gk = C.tz_g.next()
        P.add("act", lambda e, g=g, ps=ps: e.copy(out=g[:], in_=ps[:]), r=[psk], w=[gk])
        dma(P, "sp", bass.AP(g2_t, slot * 128 * GW + cc * 512, [[GW, 128], [1, 512]]), g[:], r=[gk], w=[(g2k, cc)])


def emit_toeplitz_load(C, g2_t, slot, width=TW):
    nc, P = C.nc, C.P
    if not hasattr(C, "tz_st"):
        C.tz_st = Rot(nc, "tz_st", 2, [128, 560], F32)
        C.tz_i = 0
    g2k = ("g2", slot)
    T = C.tz_T[C.tz_i % 2]
    Tk = ("tz_T", C.tz_i % 2)
    C.tz_i += 1
    nst = width // 560
    for m in range(nst):
        st, sk = C.tz_st.next()
        dma(P, "sp", st[:], bass.AP(g2_t, slot * 128 * GW + 127 + m * 560, [[GW - 1, 128], [1, 560]]),
            r=[(g2k, cc) for cc in range(GW // 512)], w=[sk])
        P.add("act", lambda e, st=st, m=m: e.activation(out=T[:, m * 560:(m + 1) * 560], in_=st[:], func=AF.Exp), r=[sk], w=[(Tk, m)] + list(C.tz_alias_keys))
    return T, [(Tk, m) for m in range(nst)]


def emit_attention(C, mode, QT, KT, qk_keys, V_of, v_keys, scale, oT_dst, T=None, T_keys=(), negmT=None, negm_key=None, cmask=None, dv=64, bp=None):
    nc, P = C.nc, C.P
    if not hasattr(C, "at_p"):
        C.at_p = Rot(nc, "at_p", 4, [128, 512], BF16)
        C.at_rec = Rot(nc, "at_rec", 2, [65, 512], F32)
        C.at_bc = Rot(nc, "at_bc", 2, [64, 512], F32)
        C.at_o = Rot(nc, "at_o", 2, [128, 512], BF16)
        C.at_z = {0: Rot(nc, "at_z0", 2, [128, 512], BF16), 64: Rot(nc, "at_z64", 2, [128, 512], BF16)}
        for b_, rot_ in C.at_z.items():
            for j_, t_ in enumerate(rot_.tiles):
                P.add("pool", lambda e, t_=t_, b_=b_: e.memset(t_[64 - b_:128 - b_, :], 0.0), r=[], w=[(rot_.name, j_)])
        C.at_sbank = 0
        C.at_obank = 0
    if mode == "sb" and not hasattr(C, "sb_e"):
        C.sb_e = Rot(nc, "sb_e", 2, [128, 512], F32)
        C.sb_sp = Rot(nc, "sb_sp", 2, [128, 512], BF16)
        C.sb_arg = Rot(nc, "sb_arg", 3, [128, 512], F32)
        C.sb_B = salloc(nc, "sb_B", [128, 512], F32)
        C.sb_negU = salloc(nc, "sb_negU", [128, 128], BF16)
        C.sb_negO = salloc(nc, "sb_negO", [128, 128], BF16)
        C.sb_zero = salloc(nc, "sb_zero", [128, 512], BF16)
        P.add("pool", lambda e: e.memset(C.sb_negO[:], -1.0), r=[], w=["sb_negO"])
        P.add("pool", lambda e: e.memset(C.sb_zero[:], 0.0), r=[], w=["sb_zero"])
        P.add("dve", lambda e: e.tensor_copy(out=C.sb_negU[:], in_=C.negU_f[:]), r=["negU_f"], w=["sb_negU"])
    for qg in range(8):
        t0 = qg * 512
        kt_hi = 4 * qg + 3
        kt_lo = max(0, (t0 - 2048) // 128) if mode == "dil" else 0
        kts = list(range(kt_lo, kt_hi + 1))
        if mode == "sb":
            kts = kts[::-1]
        ob = 4 + (C.at_obank % 2)
        C.at_obank += 1
        o_ps, o_psk = C.ps[ob], C.psk[ob]
        nrow = dv + (0 if mode == "sb" else 1)
        if bp is not None:
            Z, Zk = C.at_z[bp].next()
            P.add("pool", lambda e, Z=Z, t0=t0: e.tensor_copy(out=Z[bp:bp + 64, :], in_=QT[:, t0:t0 + 512]), r=list(qk_keys), w=[Zk])
            qkeys_g = [Zk] + list(qk_keys)
        else:
            Z, qkeys_g = None, list(qk_keys)
        if mode == "sb":
            P.add("pe", lambda e, o_ps=o_ps: e.matmul(o_ps[0:128, :], lhsT=C.sb_zero[:, 0:128], rhs=C.sb_zero[:], start=True, stop=False, skip_group_check=True),
                  r=["sb_zero"], w=[o_psk])
            P.add("pool", lambda e: e.memset(C.sb_B[:], 0.0), r=[], w=["sb_B"])

        def scores(kt):
            sb_ = C.at_sbank % 4
            C.at_sbank += 1
            s_ps, s_psk = C.ps[sb_], C.psk[sb_]
            s0 = kt * 128
            c0 = max(0, s0 - t0)
            diag = s0 + 127 > t0 + c0
            ops = []
            need_bias = False
            need_cm = mode in ("mla", "sb") and diag
            c1 = None
            if mode == "moba":
                n = kt // 2
                c1 = max(c0, (n + 1) * 256 - t0)
                if c1 >= 512:
                    c1 = None

            def f(e, t0=t0, Z=Z):
                last = not (need_bias or need_cm or c1 is not None)
                rq = Z[:, c0:512] if Z is not None else QT[:, t0 + c0:t0 + 512]
                ins = e.matmul(s_ps[:, c0:512], lhsT=KT[:, s0:s0 + 128], rhs=rq, start=True, stop=last, skip_group_check=True)
                if need_bias:
                    off = (t0 - s0) + 384
                    last = c1 is None
                    ins = e.matmul(s_ps[:, c0:512], lhsT=C.ident_b[:], rhs=T[:, off + c0:off + 512], start=False, stop=last, skip_group_check=True)
                if need_cm:
                    ins = e.matmul(s_ps[:, c0:c0 + 128], lhsT=C.ident_b[:], rhs=cmask[:], start=False, stop=True, skip_group_check=True)
                if c1 is not None:
                    ins = e.matmul(s_ps[:, c1:512], lhsT=C.Emat[:, kt // 2, :], rhs=negmT[:, t0 + c1:t0 + 512], start=False, stop=True, skip_group_check=True)
                return ins
            rk = list(qkeys_g) + (list(T_keys) if need_bias else []) + (["cmask"] if need_cm else []) + ([negm_key, "Emat"] if c1 is not None else []) + ["ident_b"]
            P.add("pe", f, r=rk, w=[s_psk])
            return (s_ps, s_psk, c0, s0)

        nk = len(kts)
        pend = [scores(kts[0])]
        if nk > 1:
            pend.append(scores(kts[1]))
        prev2 = None
        for i, kt in enumerate(kts):
            if i + 2 < nk:
                pend.append(scores(kts[i + 2]))
            cur = pend.pop(0)
            s_ps, s_psk, c0, s0 = cur
            pT, pTk = C.at_p.next()
            first, lastk = (i == 0), (i == nk - 1)
            if mode != "sb":
                P.add("act", lambda e, pT=pT, s_ps=s_ps, c0=c0: e.activation(out=pT[:, c0:512], in_=s_ps[:, c0:512], func=AF.Exp, scale=scale), r=[s_psk], w=[pTk])
                if mode in ("moba", "dil"):
                    off = (t0 - s0) + 384
                    P.add("dve", lambda e, pT=pT, c0=c0, off=off: e.tensor_tensor(out=pT[:, c0:512], in0=pT[:, c0:512], in1=T[:, off + c0:off + 512], op=ALU.mult),
                          r=[pTk] + list(T_keys), w=[pTk])
                P.add("pe", lambda e, pT=pT, c0=c0, kt=kt, first=first, lastk=lastk, o_ps=o_ps: e.matmul(
                    o_ps[0:nrow, c0:512], lhsT=V_of(kt), rhs=pT[:, c0:512], start=first, stop=lastk, skip_group_check=True),
                    r=[pTk] + list(v_keys), w=[o_psk])
            else:
                E, Ek = C.sb_e.next()
                SP, SPk = C.sb_sp.next()
                ARG, ARGk = C.sb_arg.next()
                P.add("act", lambda e, E=E, s_ps=s_ps, c0=c0: e.activation(out=E[:, c0:512], in_=s_ps[:, c0:512], func=AF.Exp), r=[s_psk], w=[Ek])
                P.add("act", lambda e, E=E, SP=SP, c0=c0: e.activation(out=SP[:, c0:512], in_=E[:, c0:512], func=AF.Ln, bias=C.one_col[:], scale=1.0), r=[Ek, "one_col"], w=[SPk])
                w_ps, w_psk = C.ps[6], C.psk[6]

                def fw(e, SP=SP, c0=c0, s0=s0, w_ps=w_ps, t0=t0, Z=Z):
                    rq = Z[:, c0:512] if Z is not None else QT[:, t0 + c0:t0 + 512]
                    e.matmul(w_ps[:, c0:512], lhsT=KT[:, s0:s0 + 128], rhs=rq, start=True, stop=False, skip_group_check=True)
                    ins = e.matmul(w_ps[:, c0:512], lhsT=C.sb_negU[:], rhs=SP[:, c0:512], start=False, stop=not (s0 + 127 > t0 + c0), skip_group_check=True)
                    if s0 + 127 > t0 + c0:
                        ins = e.matmul(w_ps[:, c0:c0 + 128], lhsT=C.ident_b[:], rhs=cmask[:], start=False, stop=True, skip_group_check=True)
                    return ins
                P.add("pe", fw, r=list(qkeys_g) + [SPk, "sb_negU", "cmask", "ident_b"], w=[w_psk])
                P.add("dve", lambda e, ARG=ARG, w_ps=w_ps, c0=c0: e.tensor_tensor(out=ARG[:, c0:512], in0=w_ps[:, c0:512], in1=C.sb_B[:, c0:512], op=ALU.add),
                      r=[w_psk, "sb_B"], w=[ARGk])
                if not lastk:
                    c_ps, c_psk = C.ps[7], C.psk[7]
                    P.add("pe", lambda e, SP=SP, c0=c0, c_ps=c_ps: e.matmul(c_ps[:, c0:512], lhsT=C.sb_negO[:], rhs=SP[:, c0:512], start=True, stop=True),
                          r=[SPk, "sb_negO"], w=[c_psk])
                    P.add("dve", lambda e, c_ps=c_ps, c0=c0: e.tensor_tensor(out=C.sb_B[:, c0:512], in0=c_ps[:, c0:512], in1=C.sb_B[:, c0:512], op=ALU.add),
                          r=[c_psk, "sb_B"], w=["sb_B"])

                def stage2(pT=pT, pTk=pTk, ARG=ARG, ARGk=ARGk, c0=c0, kt=kt, lastk=lastk, o_ps=o_ps):
                    P.add("act", lambda e: e.activation(out=pT[:, c0:512], in_=ARG[:, c0:512], func=AF.Exp), r=[ARGk], w=[pTk])
                    P.add("pe", lambda e: e.matmul(o_ps[0:128, c0:512], lhsT=V_of(kt), rhs=pT[:, c0:512], start=False, stop=lastk, skip_group_check=True),
                          r=[pTk] + list(v_keys), w=[o_psk])
                if prev2 is not None:
                    prev2()
                prev2 = stage2
        if prev2 is not None:
            prev2()
        oT, oTk = C.at_o.next()
        orow = 0
        if mode == "sb":
            orow = bp
            P.add("act", lambda e, oT=oT, o_ps=o_ps: e.copy(out=oT[bp:bp + 64, :], in_=o_ps[bp:bp + 64, :]), r=[o_psk], w=[oTk])
        else:
            rec, reck = C.at_rec.next()
            P.add("dve", lambda e, rec=rec, o_ps=o_ps: e.reciprocal(out=rec[64:65, :], in_=o_ps[64:65, :]), r=[o_psk], w=[reck])
            b_ps, b_psk = C.ps[7], C.psk[7]
            P.add("pe", lambda e, rec=rec, b_ps=b_ps: e.matmul(b_ps[0:64, :], lhsT=C.ones_f[64:65, 0:64], rhs=rec[64:65, :], start=True, stop=True),
                  r=[reck, "ones_f"], w=[b_psk])
            bc, bck = C.at_bc.next()
            P.add("act", lambda e, bc=bc, b_ps=b_ps: e.copy(out=bc[:], in_=b_ps[0:64, :]), r=[b_psk], w=[bck])
            P.add("dve", lambda e, oT=oT, o_ps=o_ps, bc=bc: e.tensor_tensor(out=oT[0:64, :], in0=o_ps[0:64, :], in1=bc[:], op=ALU.mult), r=[o_psk, bck], w=[oTk])
        dst, dk = oT_dst(t0)
        dma(P, "sp", dst, oT[orow:orow + 64, :], r=[oTk], w=[dk])


def load_cast(C, dst_ap, src_d, shape, key, q="pool", eng="pool"):
    nc, P = C.nc, C.P
    nm = "lc_" + "x".join(str(v) for v in shape)
    if not hasattr(C, nm):
        setattr(C, nm, Rot(nc, nm, 1 if shape[0] == 128 and len(shape) == 3 else 2, list(shape), F32))
    st, sk = getattr(C, nm).next()
    dma(P, q, st[:], src_d, r=[], w=[sk])
    P.add(eng, lambda e: e.tensor_copy(out=dst_ap, in_=st[:]), r=[sk], w=[key])


def build_attn_even():
    nc = bass.Bass("TRN2", target_bir_lowering=False)
    P = Prog(nc)
    C = setup_common(nc, P)
    din = lambda name, shape, dt=F32: nc.dram_tensor(name, shape, dt, kind="ExternalInput").ap()
    hT_all = [din("hT_allA", [2, 512, 2048], BF16), din("hT_allB", [2, 512, 2048], BF16)]
    w_in_t = din("w_in_t", [12, 128, 8, 128])
    relb = din("relb33", [33, 8])
    oh_d = din("onehot", [2, 33, GW])
    emat_d = din("emat", [128, 16 * 128], BF16)
    oT_src = nc.dram_tensor("oT_src", [512, S], BF16, kind="ExternalOutput").ap()
    g2 = nc.dram_tensor("g2", [8 * 128 * GW], F32)
    import os
    if os.environ.get("DBG"):
        dout = lambda name, shape, dt=F32: nc.dram_tensor(name, shape, dt, kind="ExternalOutput").ap()
        C.dbg = {"T": dout("dbg_T", [128, TW], BF16), "negmT": dout("dbg_negmT", [16, S], BF16), "QT": dout("dbg_QT", [128, S], BF16),
                 "KT": dout("dbg_KT", [128, S], BF16), "V": dout("dbg_V", [128, 32 * 8 * 65], BF16)}
        C.dbg_head = int(os.environ["DBG"])
    emit_attn_even(C, hT_all, w_in_t, relb, oh_d, emat_d, oT_src, g2, din("gmask", [128, 512]))
    P.emit()
    return nc


def emit_attn_even(C, hT_all, w_in_t, relb, oh_d, emat_d, oT_src, g2, gmask_d, build_g2=True):
    nc, P = C.nc, C.P
    if not hasattr(C, "win_b"):
        scr = salloc(nc, "scr_e", [128, 8 * 1536], BF16)
        C.win_b = scr[:].rearrange("p (k n) -> p k n", k=8)
        C.tz_T = [scr[:, 0:TW], scr[:, TW:2 * TW]]
        C.tz_alias_keys = [("win_b", i) for i in range(12)]
        C.QT = [salloc(nc, f"QT{i}", [128, S], BF16) for i in range(4)]
        C.KT = [salloc(nc, f"KT{i}", [128, S], BF16) for i in range(4)]
        C.Vaug = salloc(nc, "Vaug", [128, 32, 8, 65], BF16)
        C.hTa = Rot(nc, "hTa", 2, [128, 8, 512], BF16)
        C.Emat = salloc(nc, "Emat", [128, 16, 128], BF16)
        dma(P, "sp", C.Emat[:].rearrange("p a b -> p (a b)"), emat_d, r=[], w=["Emat"])
        C.relb = salloc(nc, "relb", [33, 8], F32)
        C.negmT_rot = Rot(nc, "negmT", 2, [128, S], BF16)
        for j_, t_ in enumerate(C.negmT_rot.tiles):
            P.add("pool", lambda e, t_=t_: e.memset(t_[:, :], 0.0), r=[], w=[("negmT", j_)])
        C.gG = salloc(nc, "gG", [128, 512], F32)
        C.gG2 = salloc(nc, "gG2", [128, 512], F32)
        C.gEQ = salloc(nc, "gEQ", [128, 512], F32)
        C.gm3 = salloc(nc, "gm3", [128, 32], F32)
        C.gmask = salloc(nc, "gmask", [128, 512], F32)
        dma(P, "sp", C.gmask[:], gmask_d, r=[], w=["gmask"])
        C.kms = salloc(nc, "kms", [128, 16], F32)
        C.kmb = salloc(nc, "kmb", [128, 16], BF16)
        P.add("pool", lambda e: e.memset(C.Vaug[:, :, :, 64:65], 1.0), r=[], w=["Vones"])
    dma(P, "sp", C.relb[:], relb, r=[], w=["relb"])
    if build_g2:
        for hi in range(8):
            emit_toeplitz_build(C, g2, hi, C.relb[:, hi:hi + 1], "relb", oh_d[0 if hi < 4 else 1])
    for i in range(12):
        load_cast(C, C.win_b[:, :, i * 128:(i + 1) * 128], w_in_t[i], [128, 8, 128], ("win_b", i))
    wk_all = [("win_b", i) for i in range(12)]
    bank = 0
    for tg in range(8):
        hT, hTk = C.hTa.next()
        for hp in range(2):
            dma(P, "sp", hT[:, 4 * hp:4 * hp + 4, :], hT_all[hp][tg // 4, :, (tg % 4) * 512:(tg % 4 + 1) * 512].rearrange("(k p) n -> p k n", p=128), r=["hT_all"], w=[(hTk, hp)])
        for fo in range(8):
            ps, psk = C.ps[bank % 4], C.psk[bank % 4]
            bank += 1

            def f(e, fo=fo, ps=ps, hT=hT):
                ins = None
                for k in range(8):
                    ins = e.matmul(ps[:], lhsT=C.win_b[:, k, fo * 128:(fo + 1) * 128], rhs=hT[:, k, :], start=(k == 0), stop=(k == 7))
                return ins
            P.add("pe", f, r=[(hTk, 0), (hTk, 1), ("win_b", fo)], w=[psk])
            if fo < 4:
                P.add("act", lambda e, fo=fo, ps=ps, tg=tg: e.activation(out=C.QT[fo][:, tg * 512:(tg + 1) * 512], in_=ps[:], func=AF.Copy, scale=0.125),
                      r=[psk], w=[("QT", fo, tg)])
            else:
                P.add("dve", lambda e, fo=fo, ps=ps, tg=tg: e.tensor_copy(out=C.KT[fo - 4][:, tg * 512:(tg + 1) * 512], in_=ps[:]), r=[psk], w=[("KT", fo - 4, tg)])
        for tt in range(4):
            ps, psk = C.ps[bank % 4], C.psk[bank % 4]
            bank += 1

            def f(e, tt=tt, ps=ps, hT=hT):
                ins = None
                for k in range(8):
                    ins = e.matmul(ps[:], lhsT=hT[:, k, tt * 128:(tt + 1) * 128], rhs=C.win_b[:, k, 1024:1536], start=(k == 0), stop=(k == 7))
                return ins
            P.add("pe", f, r=[(hTk, 0), (hTk, 1)] + wk_all[8:12], w=[psk])
            P.add("act", lambda e, ps=ps, tg=tg, tt=tt: e.copy(out=C.Vaug[:, tg * 4 + tt, :, 0:64], in_=ps[:].rearrange("p (h d) -> p h d", h=8)),
                  r=[psk], w=[("V", tg * 4 + tt)])
    vkeys = [("V", i) for i in range(32)] + ["Vones"]
    heads = []
    for hi in range(8):
        pair, bp = hi // 2, (hi % 2) * 64
        heads.append((hi, pair, bp, C.QT[pair][bp:bp + 64, :], C.KT[pair][bp:bp + 64, :],
                      [("QT", pair, tg) for tg in range(8)] + [("KT", pair, tg) for tg in range(8)]))

    def gating(hi, pair, bp, QTh, KTh, qk_keys):
        negmT, nmk = C.negmT_rot.next()
        P.add("dve", lambda e: e.tensor_reduce(out=C.kms[bp:bp + 64, :], in_=KTh.rearrange("p (n l) -> p n l", l=256), axis=AX.X, op=ALU.add),
              r=qk_keys[8:], w=["kms"])
        P.add("dve", lambda e: e.tensor_scalar(out=C.kmb[bp:bp + 64, :], in0=C.kms[bp:bp + 64, :], scalar1=1.0 / 256, scalar2=None, op0=ALU.mult),
              r=["kms"], w=["kmb"])
        g_ps, g_psk = C.ps[6], C.psk[6]

        def fg(e):
            ins = None
            for qt in range(32):
                ins = e.matmul(g_ps[:, qt * 16:(qt + 1) * 16], lhsT=QTh[:, qt * 128:(qt + 1) * 128], rhs=C.kmb[bp:bp + 64, :], start=True, stop=True)
            return ins
        P.add("pe", fg, r=qk_keys[:8] + ["kmb"], w=[g_psk])
        G, G2, EQ, m = C.gG, C.gG2, C.gEQ, C.gm3
        v3 = lambda t: t[:].rearrange("p (q n) -> p q n", n=16)
        bc = lambda t: t[:].unsqueeze(2).to_broadcast([128, 32, 16])
        P.add("dve", lambda e: e.tensor_tensor(out=G[:], in0=g_ps[:], in1=C.gmask[:], op=ALU.add), r=[g_psk, "gmask"], w=["gG"])
        src, srck = G, "gG"
        for it in range(2):
            P.add("dve", lambda e, src=src: e.tensor_reduce(out=m[:], in_=v3(src), axis=AX.X, op=ALU.max), r=[srck], w=["gm3"])
            P.add("dve", lambda e, src=src: e.tensor_tensor(out=v3(EQ), in0=v3(src), in1=bc(m), op=ALU.is_ge), r=[srck, "gm3"], w=["gEQ"])
            P.add("dve", lambda e, src=src: e.scalar_tensor_tensor(out=G2[:], in0=EQ[:], scalar=-3e30, in1=src[:], op0=ALU.mult, op1=ALU.add),
                  r=["gEQ", srck], w=["gG2"])
            src, srck = G2, "gG2"
        P.add("dve", lambda e: e.tensor_reduce(out=m[:], in_=v3(G2), axis=AX.X, op=ALU.max), r=["gG2"], w=["gm3"])
        P.add("dve", lambda e: e.tensor_scalar(out=m[:], in0=m[:], scalar1=-1e29, scalar2=None, op0=ALU.max), r=["gm3"], w=["gm3"])
        P.add("dve", lambda e: e.tensor_tensor(out=v3(EQ), in0=v3(G), in1=bc(m), op=ALU.is_ge), r=["gG", "gm3"], w=["gEQ"])
        P.add("dve", lambda e: e.tensor_scalar(out=EQ[:], in0=EQ[:], scalar1=-1.0, scalar2=None, op0=ALU.add), r=["gEQ"], w=["gEQ"])
        for grp in range(8):
            t_ps, t_psk = C.ps[7], C.psk[7]
            q0 = 2 if grp == 0 else 0

            def ft(e, grp=grp, q0=q0, t_ps=t_ps):
                ins = None
                for j in range(q0, 4):
                    qt = grp * 4 + j
                    ins = e.transpose(t_ps[0:16, j * 128:(j + 1) * 128], EQ[:, qt * 16:(qt + 1) * 16], C.ident_f[:])
                return ins
            P.add("pe", ft, r=["gEQ", "ident_f"], w=[t_psk])
            P.add("act", lambda e, grp=grp, q0=q0, t_ps=t_ps: e.copy(out=negmT[0:16, grp * 512 + q0 * 128:(grp + 1) * 512], in_=t_ps[0:16, q0 * 128:512]),
                  r=[t_psk], w=[nmk])
        return negmT, nmk

    nxtT = emit_toeplitz_load(C, g2, 0)
    nxtG = gating(*heads[0])
    for (hi, pair, bp, QTh, KTh, qk_keys) in heads:
        is_a = hi < 4
        T, Tkeys = nxtT
        negmT, nmk = nxtG if is_a else (None, None)
        if hi + 1 < 8:
            nxtT = emit_toeplitz_load(C, g2, hi + 1)
            if hi + 1 < 4:
                nxtG = gating(*heads[hi + 1])

        def oT_dst(t0, hi=hi):
            return oT_src[hi * 64:(hi + 1) * 64, t0:t0 + 512], "oT_src"
        emit_attention(C, "moba" if is_a else "dil", QTh, C.KT[pair][:, :], qk_keys, lambda kt, hi=hi: C.Vaug[:, kt, hi, :], vkeys, 1.0, oT_dst,
                       T=T, T_keys=Tkeys, negmT=negmT, negm_key=nmk, bp=bp)


def gate_mask_const():
    m = np.zeros((32, 16), np.float32)
    for qt in range(32):
        m[qt, qt // 2:] = -1e30
    return np.ascontiguousarray(np.broadcast_to(m.reshape(1, 512), (128, 512)))


def prep_even_core(ab_w_in, rel_bias, half):
    w = np.asarray(ab_w_in, np.float32)
    qa, ka, va, qb, kb, vb = [w[:, i * 512:(i + 1) * 512] for i in range(6)]
    hs = slice(half * 256, (half + 1) * 256)
    cols = np.concatenate([qa[:, hs], qb[:, hs], ka[:, hs], kb[:, hs], va[:, hs], vb[:, hs]], axis=1)
    w_in_t = np.ascontiguousarray(cols.reshape(8, 128, 12, 128).transpose(2, 1, 0, 3))
    rb = np.asarray(rel_bias, np.float32)
    own = np.concatenate([rb[4 * half:4 * half + 4], rb[8 + 4 * half:8 + 4 * half + 4]], 0)
    relb33 = np.concatenate([own.T, np.ones((1, 8), np.float32)], 0)
    return {"w_in_t": w_in_t, "relb33": np.ascontiguousarray(relb33)}


def emat_const():
    e = np.zeros((128, 16, 128), np.float32)
    for n in range(16):
        e[n, n, :] = -NEG
    return e.reshape(128, 2048).astype(NPBF)


def build_attn_odd():
    nc = bass.Bass("TRN2", target_bir_lowering=False)
    P = Prog(nc)
    C = setup_common(nc, P)
    din = lambda name, shape, dt=F32: nc.dram_tensor(name, shape, dt, kind="ExternalInput").ap()
    T = {"hT_all": [din("hT_allA", [2, 512, 2048], BF16), din("hT_allB", [2, 512, 2048], BF16)], "w_in_t": din("w_in_t", [12, 128, 8, 128]), "wuq_t": din("wuq_t", [128, 2, 768]),
         "wukv_t": din("wukv_t", [128, 2, 768]), "qkvg": din("qkvg", [128, 4]), "cos_t": din("cos_t", [64, S]), "sin_t": din("sin_t", [64, S]),
         "cmasks": din("cmasks", [128, 2, 128]), "negU": din("negU", [128, 128])}
    oT_src = nc.dram_tensor("oT_src", [512, S], BF16, kind="ExternalOutput").ap()
    emit_attn_odd(C, T, oT_src)
    print("odd SBUF peak", C.A.peak)
    P.emit()
    return nc


def emit_attn_odd(C, T, oT_src):
    nc, P = C.nc, C.P
    hT_all = T["hT_all"]
    cm = salloc(nc, "cm", [128, 2, 128], BF16)
    load_cast(C, cm[:], T["cmasks"], [128, 2, 128], "cmask", q="sp", eng="dve")
    C.negU_f = salloc(nc, "negU_f", [128, 128], F32)
    dma(P, "sp", C.negU_f[:], T["negU"], r=[], w=["negU_f"])
    ones_b = salloc(nc, "ones_b", [128, 128], BF16)
    P.add("pool", lambda e: e.memset(ones_b[:], 1.0), r=[], w=["ones_b"])
    hTa = Rot(nc, "hTa", 2, [128, 8, 512], BF16)
    win_b = salloc(nc, "win_b", [128, 8, 768], BF16)
    C0 = C
    C = scope_open(C0)
    for i in range(6):
        load_cast(C, win_b[:, :, i * 128:(i + 1) * 128], T["w_in_t"][i], [128, 8, 128], ("win_b", i))
    wuq_b = salloc(nc, "wuq_b", [128, 2, 768], BF16)
    wukv_b = salloc(nc, "wukv_b", [128, 2, 768], BF16)
    for ch in range(2):
        load_cast(C, wuq_b[:, ch, :], T["wuq_t"][:, ch, :], [128, 768], ("wuq_b", ch))
        load_cast(C, wukv_b[:, ch, :], T["wukv_t"][:, ch, :], [128, 768], ("wukv_b", ch))
    wuqk = [("wuq_b", 0), ("wuq_b", 1)]
    wukvk = [("wukv_b", 0), ("wukv_b", 1)]
    gcol = salloc(nc, "qkvg", [128, 4], F32)
    dma(P, "sp", gcol[:], T["qkvg"], r=[], w=["qkvg"])
    QTc = [salloc(nc, f"QTc{i}", [128, S], BF16) for i in range(4)]
    KTc = [salloc(nc, f"KTc{i}", [128, S], BF16) for i in range(4)]
    Vc = salloc(nc, "Vc", [128, 32, 4, 65], BF16)
    P.add("pool", lambda e: e.memset(Vc[:, :, :, 64:65], 1.0), r=[], w=["Vones"])
    raw = Rot(nc, "raw", 4, [128, 512], F32)
    sq = Rot(nc, "sq", 4, [128, 512], BF16)
    rstd = Rot(nc, "rstdb", 2, [128, 512], F32)
    cn = Rot(nc, "cn", 2, [128, 4, 512], BF16)
    cs = Rot(nc, "cs", 2, [64, 2, 512], F32)
    t1 = Rot(nc, "rt1", 2, [64, 512], F32)
    t2 = Rot(nc, "rt2", 2, [64, 512], F32)
    rb = [0]

    def nbank():
        b = rb[0] % 6
        rb[0] += 1
        return C.ps[b], C.psk[b]

    def mmk(ps, lhs_of, rhs_of, n, rows=None, cols=None):
        def f(e):
            ins = None
            for k in range(n):
                out = ps[:] if rows is None else ps[rows[0]:rows[1], cols[0]:cols[1]]
                ins = e.matmul(out, lhsT=lhs_of(k), rhs=rhs_of(k), start=(k == 0), stop=(k == n - 1))
            return ins
        return f

    def rope(pre_ps, pre_k, sw_ps, sw_k, cst, csk, outs):
        a, ak = t1.next()
        b, bk = t2.next()
        P.add("dve", lambda e: e.tensor_tensor(out=a[:], in0=pre_ps[0:64, :], in1=cst[:, 0, :], op=ALU.mult), r=[pre_k] + list(csk), w=[ak])
        P.add("dve", lambda e: e.tensor_tensor(out=b[:], in0=sw_ps[0:64, :], in1=cst[:, 1, :], op=ALU.mult), r=[sw_k] + list(csk), w=[bk])
        (o0, k0) = outs[0]
        P.add("pool", lambda e: e.tensor_tensor(out=o0, in0=a[:], in1=b[:], op=ALU.add), r=[ak, bk], w=[k0])
        for (o, k) in outs[1:]:
            P.add("pool", lambda e, o=o: e.tensor_copy(out=o, in_=o0), r=[k0], w=[k])

    for tg in range(8):
        cols = slice(tg * 512, (tg + 1) * 512)
        hT, hTk = hTa.next()
        for hp in range(2):
            dma(P, "sp", hT[:, 4 * hp:4 * hp + 4, :], hT_all[hp][tg // 4, :, (tg % 4) * 512:(tg % 4 + 1) * 512].rearrange("(k p) n -> p k n", p=128), r=["hT_all"], w=[(hTk, hp)])
        cst, csk = cs.next()
        dma(P, "sp", cst[:, 0, :], T["cos_t"][:, cols], r=[], w=[(csk, 0)])
        dma(P, "sp", cst[:, 1, :], T["sin_t"][:, cols], r=[], w=[(csk, 1)])
        cskk = [(csk, 0), (csk, 1)]
        raws, sqs = [], []
        for ch in range(4):
            ps, psk = nbank()
            P.add("pe", mmk(ps, lambda k, ch=ch: win_b[:, k, ch * 128:(ch + 1) * 128], lambda k, hT=hT: hT[:, k, :], 8), r=[(hTk, 0), (hTk, 1), ("win_b", ch)], w=[psk])
            r_, rk = raw.next()
            s_, sk = sq.next()
            P.add("act", lambda e, r_=r_, ps=ps: e.copy(out=r_[:], in_=ps[:]), r=[psk], w=[rk])
            P.add("act", lambda e, s_=s_, ps=ps: e.activation(out=s_[:], in_=ps[:], func=AF.Square), r=[psk], w=[sk])
            raws.append((r_, rk))
            sqs.append((s_, sk))
        cnt, cnk = cn.next()
        for which in range(2):
            ss_ps, ss_k = C.ps[6 + which], C.psk[6 + which]
            (sa, sak), (sb2, sbk) = sqs[2 * which], sqs[2 * which + 1]
            P.add("pe", lambda e, ss_ps=ss_ps, sa=sa, sb2=sb2: (e.matmul(ss_ps[:], lhsT=ones_b[:], rhs=sa[:], start=True, stop=False),
                                                               e.matmul(ss_ps[:], lhsT=ones_b[:], rhs=sb2[:], start=False, stop=True))[1],
                  r=[sak, sbk, "ones_b"], w=[ss_k])
            rs, rsk = rstd.next()
            P.add("act", lambda e, rs=rs, ss_ps=ss_ps: e.activation(out=rs[:], in_=ss_ps[:], func=AF.Ln, bias=C.eps_col[:], scale=1.0 / 256), r=[ss_k, "eps"], w=[rsk])
            P.add("act", lambda e, rs=rs: e.activation(out=rs[:], in_=rs[:], func=AF.Exp, scale=-0.5), r=[rsk], w=[rsk])
            for c2 in range(2):
                ch = 2 * which + c2
                (r_, rk) = raws[ch]
                P.add("dve", lambda e, r_=r_, ch=ch, rs=rs, cnt=cnt: e.scalar_tensor_tensor(out=cnt[:, ch, :], in0=r_[:], scalar=gcol[:, ch:ch + 1], in1=rs[:],
                                                                                         op0=ALU.mult, op1=ALU.mult), r=[rk, rsk, "qkvg"], w=[(cnk, ch)])
        cqk = [(cnk, 0), (cnk, 1)]
        ckvk = [(cnk, 2), (cnk, 3)]
        kr_ps, kr_k = nbank()
        P.add("pe", mmk(kr_ps, lambda k: win_b[:, k, 512:576], lambda k, hT=hT: hT[:, k, :], 8, rows=(0, 64), cols=(0, 512)), r=[(hTk, 0), (hTk, 1), ("win_b", 4)], w=[kr_k])
        krs_ps, krs_k = nbank()
        P.add("pe", mmk(krs_ps, lambda k: win_b[:, k, 640:704], lambda k, hT=hT: hT[:, k, :], 8, rows=(0, 64), cols=(0, 512)), r=[(hTk, 0), (hTk, 1), ("win_b", 5)], w=[krs_k])
        rope(kr_ps, kr_k, krs_ps, krs_k, cst, cskk, [(KTc[h][0:64, cols], ("KTc", h, tg, 0)) for h in range(4)])
        for h in range(4):
            pre_ps, pre_k = nbank()
            P.add("pe", mmk(pre_ps, lambda k, h=h: wuq_b[:, k, h * 192:h * 192 + 128], lambda k, cnt=cnt: cnt[:, k, :], 2), r=cqk + wuqk, w=[pre_k])
            sw_ps, sw_k = nbank()
            P.add("pe", mmk(sw_ps, lambda k, h=h: wuq_b[:, k, h * 192 + 128:h * 192 + 192], lambda k, cnt=cnt: cnt[:, k, :], 2, rows=(0, 64), cols=(0, 512)),
                  r=cqk + wuqk, w=[sw_k])
            P.add("act", lambda e, h=h, pre_ps=pre_ps, cols=cols: e.copy(out=QTc[h][64:128, cols], in_=pre_ps[64:128, :]), r=[pre_k], w=[("QTc", h, tg, 1)])
            rope(pre_ps, pre_k, sw_ps, sw_k, cst, cskk, [(QTc[h][0:64, cols], ("QTc", h, tg, 0))])
            kn_ps, kn_k = nbank()
            P.add("pe", mmk(kn_ps, lambda k, h=h: wukv_b[:, k, h * 128:(h + 1) * 128], lambda k, cnt=cnt: cnt[:, 2 + k, :], 2), r=ckvk + wukvk, w=[kn_k])
            P.add("act", lambda e, h=h, kn_ps=kn_ps, cols=cols: e.copy(out=KTc[h][64:128, cols], in_=kn_ps[64:128, :]), r=[kn_k], w=[("KTc", h, tg, 1)])
        for tt in range(4):
            v_ps, v_k = nbank()
            P.add("pe", mmk(v_ps, lambda k, tt=tt, cnt=cnt: cnt[:, 2 + k, tt * 128:(tt + 1) * 128], lambda k: wukv_b[:, k, 512:768], 2, rows=(0, 128), cols=(0, 256)),
                  r=ckvk + wukvk, w=[v_k])
            P.add("act", lambda e, v_ps=v_ps, tg=tg, tt=tt: e.copy(out=Vc[:, tg * 4 + tt, :, 0:64], in_=v_ps[:, 0:256].rearrange("p (h d) -> p h d", h=4)),
                  r=[v_k], w=[("Vc", tg * 4 + tt)])
    vkeys = [("Vc", i) for i in range(32)] + ["Vones"]
    for h in range(4):
        qk_keys = [("QTc", h, tg, j) for tg in range(8) for j in range(2)] + [("KTc", h, tg, j) for tg in range(8) for j in range(2)]
        emit_attention(C, "mla", QTc[h], KTc[h], qk_keys, lambda kt, h=h: Vc[:, kt, h, :], vkeys, 96 ** -0.5,
                       lambda t0, h=h: (oT_src[h * 64:(h + 1) * 64, t0:t0 + 512], "oT_src"), cmask=cm[:, 0, :])
    scope_close(C)
    C = scope_open(C0)
    for i in range(6):
        load_cast(C, win_b[:, :, i * 128:(i + 1) * 128], T["w_in_t"][6 + i], [128, 8, 128], ("win_b", i))
    QTd = [salloc(nc, f"QTd{i}", [128, S], BF16) for i in range(2)]
    KTd = [salloc(nc, f"KTd{i}", [128, S], BF16) for i in range(2)]
    Vd = salloc(nc, "Vd", [128, 32, 4, 64], BF16)
    bank = 0
    for tg in range(8):
        hT, hTk = hTa.next()
        for hp in range(2):
            dma(P, "sp", hT[:, 4 * hp:4 * hp + 4, :], hT_all[hp][tg // 4, :, (tg % 4) * 512:(tg % 4 + 1) * 512].rearrange("(k p) n -> p k n", p=128), r=["hT_all"], w=[(hTk, hp)])
        for fo in range(4):
            ps, psk = C.ps[bank % 4], C.psk[bank % 4]
            bank += 1
            P.add("pe", mmk(ps, lambda k, fo=fo: win_b[:, k, fo * 128:(fo + 1) * 128], lambda k, hT=hT: hT[:, k, :], 8), r=[(hTk, 0), (hTk, 1), ("win_b", fo)], w=[psk])
            if fo < 2:
                P.add("act", lambda e, fo=fo, ps=ps, tg=tg: e.activation(out=QTd[fo][:, tg * 512:(tg + 1) * 512], in_=ps[:], func=AF.Copy, scale=0.125),
                      r=[psk], w=[("QTd", fo, tg)])
            else:
                P.add("dve", lambda e, fo=fo, ps=ps, tg=tg: e.tensor_copy(out=KTd[fo - 2][:, tg * 512:(tg + 1) * 512], in_=ps[:]), r=[psk], w=[("KTd", fo - 2, tg)])
        for tt in range(4):
            ps, psk = C.ps[bank % 4], C.psk[bank % 4]
            bank += 1
            P.add("pe", mmk(ps, lambda k, tt=tt, hT=hT: hT[:, k, tt * 128:(tt + 1) * 128], lambda k: win_b[:, k, 512:768], 8, rows=(0, 128), cols=(0, 256)),
                  r=[(hTk, 0), (hTk, 1), ("win_b", 4), ("win_b", 5)], w=[psk])
            P.add("act", lambda e, ps=ps, tg=tg, tt=tt: e.copy(out=Vd[:, tg * 4 + tt, :, :], in_=ps[:, 0:256].rearrange("p (h d) -> p h d", h=4)),
                  r=[psk], w=[("Vd", tg * 4 + tt)])
    vkeys = [("Vd", i) for i in range(32)]
    for h in range(4):
        pair, bp = h // 2, (h % 2) * 64
        qk_keys = [("QTd", pair, tg) for tg in range(8)] + [("KTd", pair, tg) for tg in range(8)]
        emit_attention(C, "sb", QTd[pair][bp:bp + 64, :], KTd[pair][:, :], qk_keys,
                       lambda kt, pair=pair: Vd[:, kt, 2 * pair:2 * pair + 2, :].rearrange("p a d -> p (a d)"), vkeys, 1.0,
                       lambda t0, h=h: (oT_src[256 + h * 64:256 + (h + 1) * 64, t0:t0 + 512], "oT_src"), cmask=cm[:, 1, :], bp=bp)
    scope_close(C)


def prep_odd_core(cd_w_in, q_g, kv_g, w_uq, w_ukv, half):
    w = np.asarray(cd_w_in, np.float32)
    z16 = np.zeros((1024, 16), np.float32)
    cq, ckv, kr = w[:, 0:256], w[:, 256:512], w[:, 512:544]
    qd, kd, vd = w[:, 544:1056], w[:, 1056:1568], w[:, 1568:2080]
    hs = slice(half * 256, (half + 1) * 256)
    x1, x2 = kr[:, 0:16], kr[:, 16:32]
    z64 = np.zeros((1024, 64), np.float32)
    kr_pre = np.concatenate([x1, z16, x2, z16, z64], 1)
    kr_sw = np.concatenate([x2, z16, x1, z16, z64], 1)
    cols = np.concatenate([cq, ckv, kr_pre, kr_sw, qd[:, hs], kd[:, hs], vd[:, hs]], 1)
    w_in_t = np.ascontiguousarray(cols.reshape(8, 128, 12, 128).transpose(2, 1, 0, 3))
    wq = np.asarray(w_uq, np.float32)
    wkv = np.asarray(w_ukv, np.float32)
    z = np.zeros((256, 16), np.float32)
    qcols, kcols, vcols = [], [], []
    for h in range(4 * half, 4 * half + 4):
        nope, pe = wq[:, h * 96:h * 96 + 64], wq[:, h * 96 + 64:h * 96 + 96]
        p1, p2 = pe[:, 0:16], pe[:, 16:32]
        qcols += [p1, z, p2, z, nope, p2, z, p1, z]
        kcols += [np.zeros((256, 64), np.float32), wkv[:, h * 128:h * 128 + 64]]
        vcols += [wkv[:, h * 128 + 64:h * 128 + 128]]
    wuq = np.concatenate(qcols, 1)
    wukv = np.concatenate(kcols + vcols, 1)
    tile2 = lambda a: np.ascontiguousarray(a.reshape(2, 128, a.shape[1]).transpose(1, 0, 2))
    g = np.concatenate([np.asarray(q_g, np.float32).reshape(2, 128).T, np.asarray(kv_g, np.float32).reshape(2, 128).T], 1)
    return {"w_in_t": w_in_t, "wuq_t": tile2(wuq), "wukv_t": tile2(wukv), "qkvg": np.ascontiguousarray(g)}


def rope_tables():
    inv = (1.0 / (np.float32(10000.0) ** (np.arange(0, 32, 2, dtype=np.float32) / np.float32(32)))).astype(np.float32)
    ang = np.arange(S, dtype=np.float32)[:, None] * inv[None, :]
    c, s_ = np.cos(ang).astype(np.float32).T, np.sin(ang).astype(np.float32).T
    cos_t = np.zeros((64, S), np.float32)
    sin_t = np.zeros((64, S), np.float32)
    cos_t[0:16], cos_t[32:48] = c, c
    sin_t[0:16], sin_t[32:48] = -s_, s_
    return cos_t, sin_t


def sb_negU():
    i = np.arange(128)
    return np.where(i[:, None] >= i[None, :], -1.0, 0.0).astype(np.float32)


_PROGS = {}


def _prog(name, fn):
    if name not in _PROGS:
        _PROGS[name] = fn()
    return _PROGS[name]


def _perm_wout(w):
    w = np.asarray(w, np.float32)
    order = np.concatenate([np.arange(r * 256, (r + 1) * 256) if t == 0 else 512 + np.arange(r * 256, (r + 1) * 256)
                            for r in range(2) for t in range(2)])
    return w[order]


def _run(nc, in_maps):
    return run_bass_kernel_spmd(nc, in_maps, core_ids=list(range(8))).results


def kernel_unfused(x, c, rel_bias, ada_w, ada_b, mix_pre_g, mix_post_g, ffn_pre_g, ffn_post_g,
                   ab_w_in, ab_w_out, cd_w_in, mla_q_norm_g, mla_kv_norm_g, mla_w_uq, mla_w_ukv, cd_w_out,
                   ffn_w_up, ffn_conv_w, ffn_conv_b, ffn_w_down):
    f32 = lambda a: np.asarray(a, np.float32)
    x, c = f32(x), f32(c)
    ident = np.eye(128, dtype=np.float32)
    cores = list(range(8))
    x_core = []
    for cid in cores:
        b, half = cid // 2, cid % 2
        xs = np.zeros((NT * 128, D), np.float32)
        if half == 0:
            xs[128:] = x[b, 0:2048]
        else:
            xs[:] = x[b, 1920:4096]
        x_core.append(xs)
    common = [{"ident_f": ident, "flags": core_flags(cid), "c_col": col_layout(c[cid // 2])} for cid in cores]
    onehot, emat = onehot_tables(), emat_const()
    cos_t, sin_t = rope_tables()
    cmasks = np.ascontiguousarray(causal_masks().transpose(1, 0, 2))
    negU = sb_negU()

    def pre_inputs(l):
        return {"gains_pre": col_layout(mix_pre_g[l]), "ada_w_t_pre": ada_w_tiles(ada_w[l]), "ada_b_col_pre": col_layout(ada_b[l])}

    def post_inputs(l):
        w_out = ab_w_out[l // 2] if l % 2 == 0 else cd_w_out[l // 2]
        return {"w_out_t": k_tiles(_perm_wout(w_out)), "w_down_t": k_tiles(ffn_w_down[l]), "w_up_t": w_up_tiles(ffn_w_up[l]),
                "conv_col": conv_cols(ffn_conv_w[l], ffn_conv_b[l]),
                "gains_post": np.ascontiguousarray(np.stack([col_layout(mix_post_g[l]), col_layout(ffn_pre_g[l]), col_layout(ffn_post_g[l])], 1)),
                "ada_w_t_post": ada_w_tiles(ada_w[l]), "ada_b_col_post": col_layout(ada_b[l])}

    oT_all = None
    for l in range(5):
        do_post, do_pre = l > 0, l < 4
        nc = _prog(("tok", do_post, do_pre), lambda: build_tok(do_post, do_pre))
        shared = {}
        if do_post:
            shared.update(post_inputs(l - 1))
        if do_pre:
            shared.update(pre_inputs(l))
        in_maps = []
        for cid in cores:
            m = dict(common[cid], x_in=x_core[cid], **shared)
            if do_post:
                m["oT_allA"] = np.ascontiguousarray(oT_all[cid // 2][:, 0:256])
                m["oT_allB"] = np.ascontiguousarray(oT_all[cid // 2][:, 256:512])
            in_maps.append(m)
        res = _run(nc, in_maps)
        if do_post:
            x_core = [res[cid]["x_out"] for cid in cores]
        if not do_pre:
            break
        hT_all = [np.ascontiguousarray(np.stack([res[2 * b]["hT_src"], res[2 * b + 1]["hT_src"]], 0)) for b in range(4)]
        if l % 2 == 0:
            nc = _prog("even", build_attn_even)
            per = [prep_even_core(ab_w_in[l // 2], rel_bias, h) for h in range(2)]
            in_maps = [dict(per[cid % 2], ident_f=ident, hT_allA=np.ascontiguousarray(hT_all[cid // 2][:, 0:512]), hT_allB=np.ascontiguousarray(hT_all[cid // 2][:, 512:1024]),
                            onehot=onehot, emat=emat, gmask=gate_mask_const()) for cid in cores]
        else:
            nc = _prog("odd", build_attn_odd)
            i = l // 2
            per = [prep_odd_core(cd_w_in[i], mla_q_norm_g[i], mla_kv_norm_g[i], mla_w_uq[i], mla_w_ukv[i], h) for h in range(2)]
            in_maps = [dict(per[cid % 2], ident_f=ident, hT_allA=np.ascontiguousarray(hT_all[cid // 2][:, 0:512]), hT_allB=np.ascontiguousarray(hT_all[cid // 2][:, 512:1024]),
                            cos_t=cos_t, sin_t=sin_t, cmasks=cmasks, negU=negU) for cid in cores]
        res = _run(nc, in_maps)
        oT_all = [np.ascontiguousarray(np.stack([res[2 * b]["oT_src"], res[2 * b + 1]["oT_src"]], 0)) for b in range(4)]
    out = np.zeros((4, S, D), np.float32)
    for cid in cores:
        b, half = cid // 2, cid % 2
        out[b, half * 2048:(half + 1) * 2048] = x_core[cid][128:]
    return out


def kernel(**inputs):
    return kernel_fused(**inputs)


PAIRS = [[0, 1], [2, 3], [4, 5], [6, 7]]


NLAYERS = [4]


def build_fused():
    NL = NLAYERS[0]
    nc = bass.Bass("TRN2", target_bir_lowering=False)
    P = Prog(nc)
    C = setup_common(nc, P)
    din = lambda name, shape, dt=F32: nc.dram_tensor(name, shape, dt, kind="ExternalInput").ap()
    x_in = din("x_in", [NT * 128, D])
    flags_d = din("flags", [128, 4])
    c_col = din("c_col", [128, 8])
    x_out = nc.dram_tensor("x_out", [NT * 128, D], F32, kind="ExternalOutput").ap()
    x_dram = nc.dram_tensor("x_dram", [NT * 128, D], F32).ap()
    hT_src = nc.dram_tensor("hT_src", [D, 2048], BF16).ap()
    hT_all2 = [nc.dram_tensor(f"hT_all{i}", [2 * 512, 2048], BF16).ap() for i in range(2)]
    oT_src = nc.dram_tensor("oT_src", [512, S], BF16).ap()
    oT_all2 = [nc.dram_tensor(f"oT_all{i}", [2 * 256, S], BF16).ap() for i in range(2)]
    g2 = nc.dram_tensor("g2", [8 * 128 * GW], F32)
    wcache = nc.dram_tensor("wcache", [NCH, 128, 8 * 256], BF16).ap()
    hT_all = [a.rearrange("(r k) n -> r k n", r=2) for a in hT_all2]
    oT_all = [a.rearrange("(r k) n -> r k n", r=2) for a in oT_all2]
    flags = salloc(nc, "flags_sb", [128, 4], F32)
    dma(P, "sp", flags[:], flags_d, r=[], w=["flags"])
    setup_cond(C, c_col)
    mods = [salloc(nc, f"modL{l}", [128, 48], F32) for l in range(4)]
    oh_d = din("onehot", [2, 33, GW])
    emat_d = din("emat", [128, 16 * 128], BF16)
    relb = din("relb33", [33, 8])
    gmask_d = din("gmask", [128, 512])
    odd_const = {"cos_t": din("cos_t", [64, S]), "sin_t": din("sin_t", [64, S]), "cmasks": din("cmasks", [128, 2, 128]), "negU": din("negU", [128, 128])}
    Tq_prev = None
    for l in range(NL + 1):
        do_post, do_pre = l > 0, l < NL
        C0 = C
        C = scope_open(C0)
        Tp = Tq = None
        if do_post:
            lp = l - 1
            Tp = {"w_out_t": din(f"L{lp}_w_out_t", [128, 8, 1024]), "w_down_t": din(f"L{lp}_w_down_t", [128, NCH, 1024]),
                  "w_up_t": din(f"L{lp}_w_up_t", [NCH, 2, 128, 8, 128]), "conv_col": din(f"L{lp}_conv_col", [128, 44, 4]),
                  "gains_post": din(f"L{lp}_gains_post", [128, 3, 8]), "mod": mods[lp], "modk": f"mod_L{lp}", "wcache": wcache}
            tok_setup_post(C, Tp, flags)
        pre_setup = None
        if do_pre:
            Tq = {"gains_pre": din(f"L{l}_gains_pre", [128, 8]), "mod": mods[l], "modk": f"mod_L{l}"}
            adw, adb = din(f"L{l}_ada_w_t", [48, 128, 8, 128]), din(f"L{l}_ada_b_col", [128, 48])

            def pre_setup(C=C, Tq=Tq, adw=adw, adb=adb, l=l):
                emit_mod(C, c_col, adw, adb, f"L{l}", mod=mods[l])
                tok_setup_pre(C, Tq)
        x_src = x_in if l <= 1 else x_dram
        x_dst = x_out if l == NL else x_dram
        emit_tok(C, Tp, Tq, x_src, x_dst, oT_all, hT_src, flags, pre_setup=pre_setup)
        if l == 0:
            relb_t = salloc(nc, "relb0", [33, 8], F32)
            dma(P, "sp", relb_t[:], relb, r=[], w=["relb0"])
            C.tz_oh = Rot(nc, "tz_oh", 4, [33, 512], F32)
            C.tz_rb = Rot(nc, "tz_rb", 2, [33, 128], F32)
            C.tz_g = Rot(nc, "tz_g", 4, [128, 512], F32)
            for hi in range(8):
                emit_toeplitz_build(C, g2, hi, relb_t[:, hi:hi + 1], "relb0", oh_d[0 if hi < 4 else 1])
        print("tok phase", l, "SBUF peak", C.A.peak, flush=True)
        scope_close(C)
        C = C0
        if not do_pre:
            break
        for i in range(2):
            P.add("pool", lambda e, i=i: e.collective_compute("AllGather", ALU.bypass, replica_groups=PAIRS, ins=[hT_src[i * 512:(i + 1) * 512, :]], outs=[hT_all2[i]]),
                  r=["hT_src"], w=["hT_all"], kind="cc")
        C = scope_open(C0)
        if l % 2 == 0:
            emit_attn_even(C, hT_all, din(f"L{l}_w_in_t", [12, 128, 8, 128]), relb, oh_d, emat_d, oT_src, g2, gmask_d, build_g2=False)
        else:
            T = dict(odd_const, hT_all=hT_all, w_in_t=din(f"L{l}_w_in_t", [12, 128, 8, 128]), wuq_t=din(f"L{l}_wuq_t", [128, 2, 768]),
                     wukv_t=din(f"L{l}_wukv_t", [128, 2, 768]), qkvg=din(f"L{l}_qkvg", [128, 4]))
            emit_attn_odd(C, T, oT_src)
        print("attn phase", l, "SBUF peak", C.A.peak, flush=True)
        scope_close(C)
        C = C0
        for i in range(2):
            P.add("pool", lambda e, i=i: e.collective_compute("AllGather", ALU.bypass, replica_groups=PAIRS, ins=[oT_src[i * 256:(i + 1) * 256, :]], outs=[oT_all2[i]]),
                  r=["oT_src"], w=["oT_all"], kind="cc")
    print("fused ops", len(P.ops), flush=True)
    P.emit()
    print("emit stats", sorted(P.stats.items()), flush=True)
    print("instrs per engine", {k: len(list(v.instructions)) if hasattr(v, "instructions") else None for k, v in {}.items()})
    return nc


def fused_inputs(x, c, rel_bias, ada_w, ada_b, mix_pre_g, mix_post_g, ffn_pre_g, ffn_post_g,
                 ab_w_in, ab_w_out, cd_w_in, mla_q_norm_g, mla_kv_norm_g, mla_w_uq, mla_w_ukv, cd_w_out,
                 ffn_w_up, ffn_conv_w, ffn_conv_b, ffn_w_down):
    f32 = lambda a: np.asarray(a, np.float32)
    x, c = f32(x), f32(c)
    shared = {"ident_f": np.eye(128, dtype=np.float32), "onehot": onehot_tables(), "emat": emat_const(), "gmask": gate_mask_const()}
    shared["cos_t"], shared["sin_t"] = rope_tables()
    shared["cmasks"] = np.ascontiguousarray(causal_masks().transpose(1, 0, 2))
    shared["negU"] = sb_negU()
    per_half = [dict(), dict()]
    for l in range(4):
        w_out = ab_w_out[l // 2] if l % 2 == 0 else cd_w_out[l // 2]
        shared[f"L{l}_w_out_t"] = k_tiles(_perm_wout(w_out))
        shared[f"L{l}_w_down_t"] = k_tiles(ffn_w_down[l])
        shared[f"L{l}_w_up_t"] = w_up_tiles(ffn_w_up[l])
        shared[f"L{l}_conv_col"] = conv_cols(ffn_conv_w[l], ffn_conv_b[l])
        shared[f"L{l}_gains_post"] = np.ascontiguousarray(np.stack([col_layout(mix_post_g[l]), col_layout(ffn_pre_g[l]), col_layout(ffn_post_g[l])], 1))
        shared[f"L{l}_gains_pre"] = col_layout(mix_pre_g[l])
        shared[f"L{l}_ada_w_t"] = ada_w_tiles(ada_w[l])
        shared[f"L{l}_ada_b_col"] = col_layout(ada_b[l])
        for h in range(2):
            if l % 2 == 0:
                pe = prep_even_core(ab_w_in[l // 2], rel_bias, h)
                per_half[h][f"L{l}_w_in_t"] = pe["w_in_t"]
                per_half[h]["relb33"] = pe["relb33"]
            else:
                i = l // 2
                po = prep_odd_core(cd_w_in[i], mla_q_norm_g[i], mla_kv_norm_g[i], mla_w_uq[i], mla_w_ukv[i], h)
                for k, v in po.items():
                    per_half[h][f"L{l}_{k}"] = v
    in_maps = []
    for cid in range(8):
        b, half = cid // 2, cid % 2
        xs = np.zeros((NT * 128, D), np.float32)
        if half == 0:
            xs[128:] = x[b, 0:2048]
        else:
            xs[:] = x[b, 1920:4096]
        in_maps.append(dict(shared, **per_half[half], x_in=xs, flags=core_flags(cid), c_col=col_layout(c[b])))
    return in_maps


def kernel_fused(**inputs):
    nc = _prog("fused", build_fused)
    res = _run(nc, fused_inputs(**inputs))
    out = np.zeros((4, S, D), np.float32)
    for cid in range(8):
        b, half = cid // 2, cid % 2
        out[b, half * 2048:(half + 1) * 2048] = res[cid]["x_out"][128:]
    return out
```

```python
import math
import numpy as np
import ml_dtypes
import concourse.bass as bass
import concourse.mybir as mybir
from concourse.bass_utils import run_bass_kernel_spmd

F32 = mybir.dt.float32
BF16 = mybir.dt.bfloat16
AF = mybir.ActivationFunctionType
ALU = mybir.AluOpType
AX = mybir.AxisListType
NPBF = ml_dtypes.bfloat16

D = 1024
S = 4096
DFF = 2816
NCH = 22
NT = 17
EPS = 1e-6
NEG = -30000.0
COMPUTE = ("pe", "act", "dve", "pool")


class Op:
    __slots__ = ("eng", "fn", "deps", "sig", "ticket", "kind", "sem", "semval", "idx", "prev_same_sem")


class Prog:
    def __init__(self, nc, same_eng_sync=True):
        self.nc = nc
        self.ops = []
        self.lastw = {}
        self.readers = {}
        self.same_eng_sync = same_eng_sync
        self.ndma = {"sp": 16, "act": 6, "pool": 8}
        self.dma_count = {"sp": 0, "act": 0, "pool": 0}
        self.cc_count = 0
        self._uid = 0
        self.fence_deps = []
        self.stats = {}
        self.fuse_waits = True

    def fence(self):
        last = {}
        for op in self.ops:
            if op.kind == "c":
                last[("c", op.eng)] = op.idx
            else:
                last[op.sem] = op.idx
        self.fence_deps = list(last.values())

    def uid(self, p="t"):
        self._uid += 1
        return f"{p}{self._uid}"

    def add(self, eng, fn, r=(), w=(), kind="c"):
        op = Op()
        op.eng, op.fn, op.kind, op.idx = eng, fn, kind, len(self.ops)
        op.sig = kind != "c"
        op.ticket = None
        deps = set()
        for k in r:
            d = self.lastw.get(k)
            if d is not None:
                deps.add(d)
        for k in w:
            d = self.lastw.get(k)
            if d is not None:
                deps.add(d)
            rd = self.readers.get(k)
            if rd:
                for e, lst in rd.items():
                    deps.update(lst)
        for k in r:
            rd = self.readers.setdefault(k, {})
            if kind == "c":
                rd[eng] = [op.idx]
            else:
                rd.setdefault("dma", []).append(op.idx)
        for k in w:
            self.lastw[k] = op.idx
            self.readers[k] = {}
        deps.update(self.fence_deps)
        deps.discard(op.idx)
        best = {}
        keep = []
        for d in deps:
            o = self.ops[d]
            if o.kind == "c":
                if o.eng not in best or best[o.eng] < d:
                    best[o.eng] = d
            else:
                keep.append(d)
        op.deps = keep + list(best.values())
        if kind == "dma":
            n = self.ndma[eng]
            j = self.dma_count[eng]
            self.dma_count[eng] += 1
            op.sem = (eng, j % n)
            op.semval = 16 * (j // n + 1)
        elif kind == "cc":
            self.cc_count += 1
            op.sem = ("cc", 0)
            op.semval = self.cc_count
        self.ops.append(op)
        return op.idx

    def emit(self):
        nc = self.nc
        ops = self.ops
        for op in ops:
            for d in op.deps:
                o = ops[d]
                if o.kind == "c":
                    if o.eng == op.eng and op.kind == "c" and (o.eng == "pe" or not self.same_eng_sync):
                        continue
                    o.sig = True
        cnt = {e: 0 for e in COMPUTE}
        for op in ops:
            if op.kind == "c" and op.sig:
                cnt[op.eng] += 1
                op.ticket = cnt[op.eng]
        names = [("c", e) for e in COMPUTE] + [("cc", 0)]
        for q, n in self.ndma.items():
            names += [(q, i) for i in range(n)]
        import contextlib
        with contextlib.ExitStack() as st:
            sems = {k: st.enter_context(nc.semaphore(f"s_{k[0]}_{k[1]}")) for k in names}
            block = st.enter_context(nc.Block())
            streams = {e: [o for o in ops if o.eng == e] for e in ("pe", "act", "dve", "pool", "sp")}

            def run(ename, e):
                waited = {}

                def wait(key, val):
                    if waited.get(key, 0) >= val:
                        return
                    waited[key] = val
                    self.stats[(ename, "wait")] = self.stats.get((ename, "wait"), 0) + 1
                    e.wait_ge(sems[key], val)

                class Rec:
                    def __init__(self):
                        self.first = None

                    def __getattr__(self, name):
                        attr = getattr(e, name)
                        if not callable(attr):
                            return attr

                        def call(*a, **k):
                            r = attr(*a, **k)
                            if self.first is None:
                                self.first = r
                            return r
                        return call

                for op in streams[ename]:
                    need = []
                    for d in sorted(op.deps):
                        o = ops[d]
                        if o.kind == "c":
                            if o.eng == ename and op.kind == "c" and (ename == "pe" or not self.same_eng_sync):
                                continue
                            need.append((("c", o.eng), o.ticket))
                        else:
                            need.append((o.sem, o.semval))
                    if op.kind == "dma" and op.semval > 16:
                        need.append((op.sem, op.semval - 16))
                    mx = {}
                    for k, v in need:
                        if waited.get(k, 0) < v and mx.get(k, 0) < v:
                            mx[k] = v
                    items = list(mx.items())
                    fused = items.pop() if (items and self.fuse_waits) else None
                    for k, v in items:
                        wait(k, v)
                    rec = Rec()
                    ins = op.fn(rec)
                    if fused is not None:
                        k, v = fused
                        waited[k] = v
                        self.stats[(ename, "fwait")] = self.stats.get((ename, "fwait"), 0) + 1
                        rec.first._wait_ge(sems[k], v)
                    self.stats[(ename, op.kind)] = self.stats.get((ename, op.kind), 0) + 1
                    if op.kind == "c":
                        if op.sig:
                            ins.then_inc(sems[("c", ename)], 1)
                    elif op.kind == "dma":
                        ins.then_inc(sems[op.sem], 16)
                    else:
                        ins.then_inc(sems[op.sem], 1)
                last = {}
                for op in streams[ename]:
                    if op.kind != "c":
                        last[op.sem] = max(last.get(op.sem, 0), op.semval)
                for k, v in last.items():
                    wait(k, v)

            block.tensor(lambda e: run("pe", e))
            block.scalar(lambda e: run("act", e))
            block.vector(lambda e: run("dve", e))
            block.gpsimd(lambda e: run("pool", e))
            block.sync(lambda e: run("sp", e))


_DTSIZE = {F32: 4, BF16: 2}
_ARENA = [None]


class Arena:
    def __init__(self, nc, nbytes=210944):
        self.t = nc.alloc_sbuf_tensor("arena", [128, nbytes], mybir.dt.uint8)
        self.off = 0
        self.size = nbytes
        self.peak = 0

    def alloc(self, shape, dtype):
        n = 1
        for v in shape[1:]:
            n *= v
        nb = (n * _DTSIZE[dtype] + 31) // 32 * 32
        assert self.off + nb <= self.size, f"SBUF arena overflow: need {nb} at {self.off}"
        v = self.t[0:shape[0], self.off:self.off + n * _DTSIZE[dtype]].bitcast(dtype)
        self.off += nb
        self.peak = max(self.peak, self.off)
        if len(shape) == 3:
            v = v.rearrange("p (a b) -> p a b", a=shape[1])
        elif len(shape) == 4:
            v = v.rearrange("p (a b c) -> p a b c", a=shape[1], b=shape[2])
        return v


def salloc(nc, name, shape, dtype):
    return _ARENA[0].alloc(list(shape), dtype)


class Rot:
    def __init__(self, nc, name, n, shape, dtype):
        self.tiles = [salloc(nc, f"{name}_{i}", shape, dtype) for i in range(n)]
        self.name = name
        self.i = 0

    def next(self):
        j = self.i % len(self.tiles)
        self.i += 1
        return self.tiles[j], (self.name, j)


def dma(P, q, out, in_, r, w, **kw):
    return P.add(q, lambda e: e.dma_start(out=out, in_=in_, **kw), r=r, w=w, kind="dma")


class Ctx:
    pass


def scope_open(C):
    import copy
    Cc = copy.copy(C)
    Cc._mark = C.A.off
    return Cc


def scope_close(Cc):
    Cc.A.off = Cc._mark
    Cc.P.fence()


def setup_common(nc, P):
    C = Ctx()
    C.nc, C.P = nc, P
    C.A = Arena(nc)
    _ARENA[0] = C.A
    C.ps = [nc.alloc_psum_tensor(f"ps{i}", [128, 512], F32) for i in range(8)]
    C.psk = [("ps", i) for i in range(8)]
    C.ident_f_d = nc.dram_tensor("ident_f", [128, 128], F32, kind="ExternalInput").ap()
    C.ident_f = salloc(nc, "ident_f_sb", [128, 128], F32)
    C.ident_b = salloc(nc, "ident_b_sb", [128, 128], BF16)
    dma(P, "sp", C.ident_f[:], C.ident_f_d, r=[], w=["ident_f"])
    C.eps_col = salloc(nc, "eps_col", [128, 1], F32)
    C.ones_f = salloc(nc, "ones_f", [128, 128], F32)
    P.add("pool", lambda e: e.memset(C.ones_f[:], 1.0), r=[], w=["ones_f"])
    C.diag_rot = Rot(nc, "diag", 2, [128, 128], F32)
    C.one_col = salloc(nc, "one_col", [128, 1], F32)
    P.add("pool", lambda e: e.memset(C.one_col[:], 1.0), r=[], w=["one_col"])
    P.add("pool", lambda e: e.memset(C.eps_col[:], EPS), r=[], w=["eps"])
    P.add("dve", lambda e: e.tensor_copy(out=C.ident_b[:], in_=C.ident_f[:]), r=["ident_f"], w=["ident_b"])
    return C


def emit_rstd(C, ss_ap, ss_key, n_feat, rot):
    P = C.P
    t, k = rot.next()
    shp = ss_ap.shape
    o = t[0:shp[0], 0:shp[1]]
    P.add("act", lambda e: e.activation(out=o, in_=ss_ap, func=AF.Ln, bias=C.eps_col[0:shp[0], :], scale=1.0 / n_feat),
          r=[ss_key, "eps"], w=[k])
    P.add("act", lambda e: e.activation(out=o, in_=o, func=AF.Exp, scale=-0.5), r=[k], w=[k])
    return o, k


def setup_cond(C, c_col_d):
    nc, P = C.nc, C.P
    C.cond = salloc(nc, "cond", [128, 8], F32)
    ccol = salloc(nc, "ccol", [128, 8], F32)
    tmp = salloc(nc, "cond_tmp", [128, 8], F32)
    dma(P, "sp", ccol[:], c_col_d, r=[], w=["ccol"])
    P.add("act", lambda e: e.activation(out=tmp[:], in_=ccol[:], func=AF.Exp, scale=-1.0), r=["ccol"], w=["cond_tmp"])
    P.add("dve", lambda e: e.tensor_scalar(out=tmp[:], in0=tmp[:], scalar1=1.0, scalar2=None, op0=ALU.add), r=["cond_tmp"], w=["cond_tmp"])
    P.add("dve", lambda e: e.reciprocal(out=tmp[:], in_=tmp[:]), r=["cond_tmp"], w=["cond_tmp"])
    P.add("dve", lambda e: e.tensor_tensor(out=C.cond[:], in0=ccol[:], in1=tmp[:], op=ALU.mult), r=["cond_tmp", "ccol"], w=["cond"])


def emit_mod(C, c_col_d, ada_w_t_d, ada_b_col_d, name, mod=None):
    nc, P = C.nc, C.P
    if not hasattr(C, "cond"):
        setup_cond(C, c_col_d)
    if not hasattr(C, "adaw_rot"):
        C.adaw_rot = Rot(nc, "adaw", 2, [128, 8, 128], F32)
    if mod is None:
        mod = salloc(nc, f"mod_{name}", [128, 48], F32)
    bcol = salloc(nc, f"adab_{name}", [128, 48], F32)
    dma(P, "sp", bcol[:], ada_b_col_d, r=[], w=[f"adab_{name}"])
    ps, psk = C.ps[7], C.psk[7]
    for c in range(48):
        wt, wk = C.adaw_rot.next()
        dma(P, "act", wt[:], ada_w_t_d[c], r=[], w=[wk])

        def f(e, wt=wt, c=c):
            ins = None
            for k in range(8):
                ins = e.matmul(ps[:, c:c + 1], lhsT=wt[:, k, :], rhs=C.cond[:, k:k + 1], start=(k == 0), stop=(k == 7))
            return ins
        P.add("pe", f, r=[wk, "cond"], w=[psk])
    P.add("dve", lambda e: e.tensor_tensor(out=mod[:], in0=ps[:, 0:48], in1=bcol[:], op=ALU.add),
          r=[psk, f"adab_{name}"], w=[f"mod_{name}"])
    return mod, f"mod_{name}"


def emit_bcast_row(C, col_ap, col_key, out_tile, out_key):
    nc, P = C.nc, C.P
    for half in range(2):
        ps, psk = C.ps[6 + half], C.psk[6 + half]
        for kk in range(4):
            k = half * 4 + kk
            dg, dk = C.diag_rot.next()
            P.add("dve", lambda e, dg=dg, k=k: e.tensor_scalar(out=dg[:], in0=C.ident_f[:], scalar1=col_ap[:, k:k + 1], scalar2=None, op0=ALU.mult),
                  r=["ident_f", col_key], w=[dk])
            P.add("pe", lambda e, dg=dg, kk=kk, ps=ps: e.matmul(ps[:, kk * 128:(kk + 1) * 128], lhsT=C.ones_f[:], rhs=dg[:], start=True, stop=True),
                  r=["ones_f", dk], w=[psk])
        P.add("act", lambda e, ps=ps, half=half: e.copy(out=out_tile[:, half * 512:(half + 1) * 512], in_=ps[:]), r=[psk], w=[out_key])


def emit_norm_T(C, x_ap, x_key, a_col, b_col, ab_key, hT_ap, hT_key):
    nc, P = C.nc, C.P
    if not hasattr(C, "n_junk"):
        C.n_junk = Rot(nc, "njunk", 1, [128, 1024], BF16)
        C.n_ss = Rot(nc, "nss", 4, [128, 1], F32)
        C.n_rs = Rot(nc, "nrs", 4, [128, 1], F32)
        C.n_xn = Rot(nc, "nxn", 2, [128, 1024], BF16)
    jt, jk = C.n_junk.next()
    ss, ssk = C.n_ss.next()
    P.add("act", lambda e: e.activation(out=jt[:], in_=x_ap, func=AF.Square, accum_out=ss[:]), r=[x_key], w=[jk, ssk])
    rs, rsk = emit_rstd(C, ss[:], ssk, D, C.n_rs)
    xn, xnk = C.n_xn.next()
    P.add("act", lambda e: e.activation(out=xn[:], in_=x_ap, func=AF.Copy, scale=rs), r=[x_key, rsk], w=[xnk])
    ps, psk = C.ps[6], C.psk[6]
    psb = ps[:].bitcast(BF16)

    def ft(e):
        ins = None
        for k in range(8):
            ins = e.transpose(psb[:, k * 128:(k + 1) * 128], xn[:, k * 128:(k + 1) * 128], C.ident_b[:])
        return ins
    P.add("pe", ft, r=[xnk, "ident_b"], w=[psk])
    for k in range(8):
        P.add("dve", lambda e, k=k: e.tensor_scalar(out=hT_ap[:, k, :], in0=psb[:, k * 128:(k + 1) * 128],
                                                     scalar1=a_col[:, k:k + 1], scalar2=b_col[:, k:k + 1], op0=ALU.mult, op1=ALU.add),
              r=[psk, ab_key], w=[hT_key])


def emit_resid_update(C, y_banks, y_keys, x_ap, x_key, gm_tile, gm_key):
    nc, P = C.nc, C.P
    if not hasattr(C, "r_junk"):
        C.r_junk = Rot(nc, "rjunk", 2, [128, 512], BF16)
        C.r_ss = Rot(nc, "rss", 4, [128, 2], F32)
        C.r_s1 = Rot(nc, "rs1", 4, [128, 1], F32)
        C.r_rs = Rot(nc, "rrs", 4, [128, 1], F32)
        C.r_tmp = Rot(nc, "rtmp", 1, [128, 1024], F32)
    ss, ssk = C.r_ss.next()
    for h in range(2):
        jt, jk = C.r_junk.next()
        P.add("act", lambda e, h=h, jt=jt: e.activation(out=jt[:], in_=y_banks[h][:], func=AF.Square, accum_out=ss[:, h:h + 1]),
              r=[y_keys[h]], w=[jk, (ssk, h)])
    s1, s1k = C.r_s1.next()
    P.add("dve", lambda e: e.tensor_tensor(out=s1[:], in0=ss[:, 0:1], in1=ss[:, 1:2], op=ALU.add), r=[(ssk, 0), (ssk, 1)], w=[s1k])
    rs, rsk = emit_rstd(C, s1[:], s1k, D, C.r_rs)
    tmp, tk = C.r_tmp.next()
    for h in range(2):
        P.add("dve", lambda e, h=h: e.scalar_tensor_tensor(out=tmp[:, h * 512:(h + 1) * 512], in0=y_banks[h][:], scalar=rs,
                                                            in1=gm_tile[:, h * 512:(h + 1) * 512], op0=ALU.mult, op1=ALU.mult),
              r=[y_keys[h], rsk, gm_key], w=[(tk, h)])
    P.add("pool", lambda e: e.tensor_tensor(out=x_ap, in0=x_ap, in1=tmp[:], op=ALU.add), r=[(tk, 0), (tk, 1), x_key], w=[x_key])


GROUPS = [(0, 1), (1, 4), (5, 4), (9, 4), (13, 4)]


def tok_setup_post(C, T, flags):
    nc, P = C.nc, C.P
    mod, modk = T["mod"], T["modk"]
    gcol = salloc(nc, P.uid("gcol"), [128, 3, 8], F32)
    gck = P.uid("gck")
    dma(P, "sp", gcol[:], T["gains_post"], r=[], w=[gck])
    small = salloc(nc, P.uid("small"), [128, 3, 8], F32)
    smk = P.uid("smk")
    P.add("dve", lambda e: e.tensor_tensor(out=small[:, 0, :], in0=mod[:, 16:24], in1=gcol[:, 0, :], op=ALU.mult), r=[modk, gck], w=[(smk, 0)])
    P.add("dve", lambda e: e.scalar_tensor_tensor(out=small[:, 1, :], in0=mod[:, 32:40], scalar=1.0, in1=gcol[:, 1, :], op0=ALU.add, op1=ALU.mult),
          r=[modk, gck], w=[(smk, 1)])
    P.add("dve", lambda e: e.tensor_tensor(out=small[:, 2, :], in0=mod[:, 40:48], in1=gcol[:, 2, :], op=ALU.mult), r=[modk, gck], w=[(smk, 2)])
    if not hasattr(C, "wout_b"):
        C.gm = salloc(nc, "gm", [128, 1024], F32)
        C.gf = salloc(nc, "gf", [128, 1024], F32)
        C.wout_b = salloc(nc, "wout_b", [128, 8, 1024], BF16)
        C.wdn_b = salloc(nc, "wdn_b", [128, NCH, 1024], BF16)
        C.wstage = Rot(nc, "wstage", 2, [128, 1024], F32)
        C.wup_b = Rot(nc, "wup_b", 2, [128, 8, 256], BF16)
        C.convc = salloc(nc, "convc", [128, 44, 4], F32)
        C.tails = salloc(nc, "tails", [128, 44, 2], F32)
        C.oa = Rot(nc, "oa", 2, [128, 8, 128], BF16)
        C.ob = Rot(nc, "ob", 2, [128, 8, 128], BF16)
        C.osel = Rot(nc, "osel", 2, [128, 8, 128], BF16)
        C.U = Rot(nc, "U", 2, [128, 514], F32)
        C.cv = Rot(nc, "cv", 4, [128, 512], F32)
        C.gl = Rot(nc, "gl", 1, [128, 512], F32)
        C.gT = salloc(nc, "gT", [128, NCH, 512], BF16)
    gmk, gfk = P.uid("gmk"), P.uid("gfk")
    emit_bcast_row(C, small[:, 0, :], (smk, 0), C.gm, "gm")
    emit_bcast_row(C, small[:, 2, :], (smk, 2), C.gf, "gf")
    for k in range(8):
        st, sk = C.wstage.next()
        dma(P, "sp", st[:], T["w_out_t"][:, k, :], r=[], w=[sk])
        P.add("act", lambda e, st=st, k=k: e.copy(out=C.wout_b[:, k, :], in_=st[:]), r=[sk], w=[("wout_b", k)])

    def load_w_down(C=C, T=T):
        for k in range(NCH):
            st, sk = C.wstage.next()
            dma(P, "sp", st[:], T["w_down_t"][:, k, :], r=[], w=[sk])
            P.add("pool", lambda e, st=st, k=k: e.tensor_copy(out=C.wdn_b[:, k, :], in_=st[:]), r=[sk], w=[("wdn_b", k)])
    T["load_w_down"] = load_w_down
    dma(P, "sp", C.convc[:], T["conv_col"], r=[], w=["convc"])
    P.add("pool", lambda e: e.memset(C.tails[:], 0.0), r=[], w=[("tails", c) for c in range(44)])
    T["small"], T["smk"] = small, smk


def tok_setup_pre(C, T):
    nc, P = C.nc, C.P
    mod, modk = T["mod"], T["modk"]
    gcol = salloc(nc, P.uid("gpre"), [128, 8], F32)
    gck = P.uid("gprek")
    dma(P, "sp", gcol[:], T["gains_pre"], r=[], w=[gck])
    am = salloc(nc, P.uid("am"), [128, 8], F32)
    amk = P.uid("amk")
    P.add("dve", lambda e: e.scalar_tensor_tensor(out=am[:], in0=mod[:, 8:16], scalar=1.0, in1=gcol[:], op0=ALU.add, op1=ALU.mult),
          r=[modk, gck], w=[amk])
    T["am"], T["amk"] = am, amk


def pipeline(stages, n):
    ns = len(stages)
    for step in range(n + ns - 1):
        for si in range(ns):
            i = step - si
            if 0 <= i < n:
                stages[si](i)


def emit_tok(C, Tp, Tq, x_src_d, x_dst_d, oT_all_d, hT_src_d, flags, pre_setup=None):
    nc, P = C.nc, C.P
    if not hasattr(C, "xg"):
        C.xg = Rot(nc, "xg", 2, [128, 4, D], F32)
        C.hT = Rot(nc, "hTf", 2, [128, 8, 512], BF16)
    woutk = [("wout_b", k) for k in range(8)]
    wdnk = [("wdn_b", k) for k in range(NCH)]
    ybank = [0]

    def next_y():
        yb = [C.ps[ybank[0]], C.ps[ybank[0] + 1]]
        ybk = [C.psk[ybank[0]], C.psk[ybank[0] + 1]]
        ybank[0] = (ybank[0] + 2) % 4
        return yb, ybk

    if Tp is None and pre_setup is not None:
        pre_setup()
        pre_setup = None
    for gi, (t0, ntl) in enumerate(GROUPS):
        N = ntl * 128
        if Tp is None and gi == 0:
            continue
        xg, xgk = C.xg.next()
        for ti in range(ntl):
            tt = t0 + ti
            dma(P, "sp", xg[:, ti, :], x_src_d[tt * 128:(tt + 1) * 128, :], r=["x_src"], w=[(xgk, ti)])
        if Tp is not None:
            small, smk, mod, modk = Tp["small"], Tp["smk"], Tp["mod"], Tp["modk"]
            hT, hTk = C.hT.next()
            ys = {}

            def stA(ti, gi=gi, t0=t0):
                tt = t0 + ti
                oa, oak = C.oa.next()
                ob, obk = C.ob.next()
                osel, osk = C.osel.next()
                cola = 0 if gi == 0 else (tt - 1) * 128
                colb = 2048 + (tt - 1) * 128
                for r in range(2):
                    for hp in range(2):
                        kk = 4 * r + 2 * hp
                        dma(P, "sp", oa[:, kk:kk + 2, :], oT_all_d[hp][r, :, cola:cola + 128].rearrange("(k p) n -> p k n", p=128), r=["oT_all"], w=[(oak, r, hp)])
                        dma(P, "act", ob[:, kk:kk + 2, :], oT_all_d[hp][r, :, colb:colb + 128].rearrange("(k p) n -> p k n", p=128), r=["oT_all"], w=[(obk, r, hp)])
                P.add("dve", lambda e: e.tensor_scalar(out=osel[:], in0=oa[:], scalar1=flags[:, 0:1], scalar2=None, op0=ALU.mult),
                      r=[(oak, r, hp) for r in range(2) for hp in range(2)] + ["flags"], w=[osk])
                P.add("dve", lambda e: e.scalar_tensor_tensor(out=osel[:], in0=ob[:], scalar=flags[:, 1:2], in1=osel[:], op0=ALU.mult, op1=ALU.add),
                      r=[(obk, r, hp) for r in range(2) for hp in range(2)] + ["flags", osk], w=[osk])
                yb, ybk = next_y()
                for h in range(2):
                    def f(e, h=h):
                        ins = None
                        for k in range(8):
                            ins = e.matmul(yb[h][:], lhsT=osel[:, k, :], rhs=C.wout_b[:, k, h * 512:(h + 1) * 512], start=(k == 0), stop=(k == 7))
                        return ins
                    P.add("pe", f, r=[osk] + woutk, w=[ybk[h]])
                ys[ti] = (yb, ybk)

            def stB(ti, xg=xg, xgk=xgk):
                yb, ybk = ys[ti]
                emit_resid_update(C, yb, ybk, xg[:, ti, :], (xgk, ti), C.gm, "gm")

            def stC(ti, xg=xg, xgk=xgk, hT=hT, hTk=hTk):
                emit_norm_T(C, xg[:, ti, :], (xgk, ti), small[:, 1, :], mod[:, 24:32], (smk, 1), hT[:, :, ti * 128:(ti + 1) * 128], (hTk, ti))
            pipeline([stA, stB, stC], ntl)
            if Tp.get("load_w_down") is not None:
                Tp.pop("load_w_down")()
            hTkeys = [(hTk, ti) for ti in range(ntl)]
            wus, cvss = {}, {}

            def stW(j, gi=gi):
                wu, wuk = C.wup_b.next()
                wc = Tp["wcache"]
                if gi == 0:
                    for half in range(2):
                        st, sk = C.wstage.next()
                        dma(P, "sp", st[:], Tp["w_up_t"][j, half].rearrange("p k n -> p (k n)"), r=[], w=[sk])
                        if half == 0:
                            P.add("dve", lambda e, st=st, half=half: e.tensor_copy(out=wu[:, :, half * 128:(half + 1) * 128], in_=st[:].rearrange("p (k n) -> p k n", k=8)),
                                  r=[sk], w=[(wuk, half)])
                        else:
                            P.add("act", lambda e, st=st, half=half: e.copy(out=wu[:, :, half * 128:(half + 1) * 128], in_=st[:].rearrange("p (k n) -> p k n", k=8)),
                                  r=[sk], w=[(wuk, half)])
                    dma(P, "sp", wc[j], wu[:].rearrange("p k n -> p (k n)"), r=[(wuk, 0), (wuk, 1)], w=[("wcache", j)])
                else:
                    dma(P, "sp", wu[:].rearrange("p k n -> p (k n)"), wc[j], r=[("wcache", j)], w=[(wuk, 0), (wuk, 1)])
                wus[j] = (wu, wuk)

            def stX(j, hT=hT, N=N, gi=gi):
                wu, wuk = wus.pop(j)
                cvs = []
                for half in range(2):
                    c = j + 22 * half
                    pb = 4 + half + 2 * (j % 2)
                    ps, psk = C.ps[pb], C.psk[pb]

                    def f(e, half=half, ps=ps):
                        ins = None
                        for k in range(8):
                            ins = e.matmul(ps[:, 0:N], lhsT=wu[:, k, half * 128:(half + 1) * 128], rhs=hT[:, k, 0:N], start=(k == 0), stop=(k == 7))
                        return ins
                    P.add("pe", f, r=[(wuk, half)] + hTkeys, w=[psk])
                    U, Uk = C.U.next()
                    P.add("act", lambda e, U=U, ps=ps: e.copy(out=U[:, 2:2 + N], in_=ps[:, 0:N]), r=[psk], w=[Uk])
                    P.add("pool", lambda e, U=U, c=c: e.tensor_copy(out=U[:, 0:2], in_=C.tails[:, c, :]), r=[("tails", c)], w=[(Uk, "t")])
                    cv, cvk = C.cv.next()
                    P.add("act", lambda e, cv=cv, ps=ps, c=c: e.activation(out=cv[:, 0:N], in_=ps[:, 0:N], func=AF.Identity,
                                                                          scale=C.convc[:, c, 2:3], bias=C.convc[:, c, 3:4]),
                          r=[psk, "convc"], w=[cvk])
                    P.add("dve", lambda e, cv=cv, U=U, c=c: e.scalar_tensor_tensor(out=cv[:, 0:N], in0=U[:, 1:1 + N], scalar=C.convc[:, c, 1:2], in1=cv[:, 0:N],
                                                                                  op0=ALU.mult, op1=ALU.add),
                          r=[Uk, (Uk, "t"), cvk, "convc"], w=[cvk])
                    P.add("dve", lambda e, cv=cv, U=U, c=c: e.scalar_tensor_tensor(out=cv[:, 0:N], in0=U[:, 0:N], scalar=C.convc[:, c, 0:1], in1=cv[:, 0:N],
                                                                                  op0=ALU.mult, op1=ALU.add),
                          r=[Uk, (Uk, "t"), cvk, "convc"], w=[cvk])
                    if gi == 0:
                        P.add("pool", lambda e, U=U, c=c: e.tensor_scalar(out=C.tails[:, c, :], in0=U[:, N:N + 2], scalar1=flags[:, 2:3], scalar2=None, op0=ALU.mult),
                              r=[Uk, "flags"], w=[("tails", c)])
                    else:
                        P.add("pool", lambda e, U=U, c=c: e.tensor_copy(out=C.tails[:, c, :], in_=U[:, N:N + 2]), r=[Uk], w=[("tails", c)])
                    cvs.append((cv, cvk))
                cvss[j] = cvs

            def stY(j, N=N):
                gl, glk = C.gl.next()
                (cg, cgk), (cvv, cvvk) = cvss.pop(j)
                P.add("act", lambda e: e.activation(out=gl[:, 0:N], in_=cg[:, 0:N], func=AF.Gelu_apprx_tanh), r=[cgk], w=[glk])
                P.add("dve", lambda e: e.tensor_tensor(out=C.gT[:, j, 0:N], in0=gl[:, 0:N], in1=cvv[:, 0:N], op=ALU.mult),
                      r=[glk, cvvk], w=[("gT", j)])
            pipeline([stW, stX, stY], NCH)
            gTk = [("gT", j) for j in range(NCH)]
            if pre_setup is not None:
                pre_setup()
                pre_setup = None
            ys2 = {}

            def stD(ti):
                yb, ybk = next_y()
                for h in range(2):
                    def f(e, h=h):
                        ins = None
                        for j in range(NCH):
                            ins = e.matmul(yb[h][:], lhsT=C.gT[:, j, ti * 128:(ti + 1) * 128], rhs=C.wdn_b[:, j, h * 512:(h + 1) * 512],
                                           start=(j == 0), stop=(j == NCH - 1))
                        return ins
                    P.add("pe", f, r=gTk + wdnk, w=[ybk[h]])
                ys2[ti] = (yb, ybk)

            def stE(ti, xg=xg, xgk=xgk):
                yb, ybk = ys2[ti]
                emit_resid_update(C, yb, ybk, xg[:, ti, :], (xgk, ti), C.gf, "gf")
            stages = [stD, stE]
            hT2 = None
            if Tq is not None and gi > 0:
                hT2, hT2k = C.hT.next()

                def stF(ti, xg=xg, xgk=xgk, hT2=hT2, hT2k=hT2k):
                    emit_norm_T(C, xg[:, ti, :], (xgk, ti), Tq["am"], Tq["mod"][:, 0:8], Tq["amk"], hT2[:, :, ti * 128:(ti + 1) * 128], (hT2k, ti))
                stages.append(stF)
            pipeline(stages, ntl)
            if hT2 is not None:
                g = gi - 1
                dma(P, "sp", hT_src_d[:, g * 512:(g + 1) * 512].rearrange("(k p) n -> p k n", p=128), hT2[:],
                    r=[(hT2k, ti) for ti in range(4)], w=["hT_src"])
        elif Tq is not None and gi > 0:
            hT, hTk = C.hT.next()
            for ti in range(ntl):
                emit_norm_T(C, xg[:, ti, :], (xgk, ti), Tq["am"], Tq["mod"][:, 0:8], Tq["amk"], hT[:, :, ti * 128:(ti + 1) * 128], (hTk, ti))
            g = gi - 1
            dma(P, "sp", hT_src_d[:, g * 512:(g + 1) * 512].rearrange("(k p) n -> p k n", p=128), hT[:],
                r=[(hTk, ti) for ti in range(4)], w=["hT_src"])
        if Tp is not None:
            for ti in range(ntl):
                tt = t0 + ti
                dma(P, "sp", x_dst_d[tt * 128:(tt + 1) * 128, :], xg[:, ti, :], r=[(xgk, ti)], w=["x_dst"])


def build_tok(do_post, do_pre):
    nc = bass.Bass("TRN2", target_bir_lowering=False)
    P = Prog(nc)
    C = setup_common(nc, P)
    din = lambda name, shape, dt=F32: nc.dram_tensor(name, shape, dt, kind="ExternalInput").ap()
    x_in = din("x_in", [NT * 128, D])
    flags_d = din("flags", [128, 4])
    c_col = din("c_col", [128, 8])
    flags = salloc(nc, "flags_sb", [128, 4], F32)
    dma(P, "sp", flags[:], flags_d, r=[], w=["flags"])
    Tp = Tq = None
    oT_all = hT_src = x_out = None
    if do_post:
        Tp = {"w_out_t": din("w_out_t", [128, 8, 1024]), "w_down_t": din("w_down_t", [128, NCH, 1024]),
              "w_up_t": din("w_up_t", [NCH, 2, 128, 8, 128]), "conv_col": din("conv_col", [128, 44, 4]),
              "gains_post": din("gains_post", [128, 3, 8]), "wcache": nc.dram_tensor("wcache", [NCH, 128, 8 * 256], BF16).ap()}
        oT_all = [din("oT_allA", [2, 256, S], BF16), din("oT_allB", [2, 256, S], BF16)]
        Tp["mod"], Tp["modk"] = emit_mod(C, c_col, din("ada_w_t_post", [48, 128, 8, 128]), din("ada_b_col_post", [128, 48]), "post")
        tok_setup_post(C, Tp, flags)
        x_out = nc.dram_tensor("x_out", [NT * 128, D], F32, kind="ExternalOutput").ap()
    if do_pre:
        Tq = {"gains_pre": din("gains_pre", [128, 8])}
        Tq["mod"], Tq["modk"] = emit_mod(C, c_col, din("ada_w_t_pre", [48, 128, 8, 128]), din("ada_b_col_pre", [128, 48]), "pre")
        tok_setup_pre(C, Tq)
        hT_src = nc.dram_tensor("hT_src", [D, 2048], BF16, kind="ExternalOutput").ap()
    emit_tok(C, Tp, Tq, x_in, x_out, oT_all, hT_src, flags)
    print("tok SBUF peak", C.A.peak)
    P.emit()
    return nc


def col_layout(v):
    return np.ascontiguousarray(np.asarray(v, np.float32).reshape(-1, 128).T)


def ada_w_tiles(w):
    return np.ascontiguousarray(np.asarray(w, np.float32).reshape(8, 128, 48, 128).transpose(2, 1, 0, 3))


def k_tiles(w):
    w = np.asarray(w, np.float32)
    return np.ascontiguousarray(w.reshape(w.shape[0] // 128, 128, w.shape[1]).transpose(1, 0, 2))


def w_up_tiles(w):
    w = np.asarray(w, np.float32)
    g = w[:, :DFF].reshape(8, 128, NCH, 128)
    v = w[:, DFF:].reshape(8, 128, NCH, 128)
    return np.ascontiguousarray(np.stack([g, v], axis=0).transpose(3, 0, 2, 1, 4))


def conv_cols(cw, cb):
    a = np.concatenate([np.asarray(cw, np.float32), np.asarray(cb, np.float32)[None]], 0)
    return np.ascontiguousarray(a.reshape(4, 44, 128).transpose(2, 1, 0))


def core_flags(core):
    half = core % 2
    f = np.zeros((128, 4), np.float32)
    f[:, 0] = 1.0 - half
    f[:, 1] = float(half)
    f[:, 2] = float(half)
    return f


TW = 4480
GW = 4608


def t5_bucket_np(d):
    d = np.maximum(d, 0)
    df = np.maximum(d, 16).astype(np.float32)
    large = 16 + (np.log(df / np.float32(16)) / np.float32(math.log(128.0)) * np.float32(16)).astype(np.int32)
    large = np.minimum(large, 31)
    return np.where(d < 16, d, large)


def onehot_tables():
    n = np.arange(GW)
    d = n - 511
    b = t5_bucket_np(d)
    oh = np.zeros((2, 33, GW), np.float32)
    va = d >= 0
    oh[0, b[va], n[va]] = 1.0
    oh[0, 32, ~va] = NEG
    cnt = ((d >= 0) & (d <= 128)).astype(np.int64) + ((d >= 0) & (d <= 512) & (d % 4 == 0)) + ((d >= 0) & (d <= 2048) & (d % 16 == 0))
    vb = cnt > 0
    oh[1, b[vb], n[vb]] = 1.0
    oh[1, 32, vb] = np.log(cnt[vb].astype(np.float64)).astype(np.float32)
    oh[1, 32, ~vb] = NEG
    return oh


def causal_masks():
    i = np.arange(128)[:, None]
    c = np.arange(128)[None, :]
    m = np.zeros((2, 128, 128), np.float32)
    m[0][c < i] = NEG
    m[1][c <= i] = NEG
    return m


def emit_toeplitz_build(C, g2_t, slot, rb_col, rb_key, oh_d):
    nc, P = C.nc, C.P
    if not hasattr(C, "tz_oh"):
        C.tz_oh = Rot(nc, "tz_oh", 2, [33, 512], F32)
        C.tz_rb = Rot(nc, "tz_rb", 2, [33, 128], F32)
        C.tz_g = Rot(nc, "tz_g", 2, [128, 512], F32)
    rb, rbk = C.tz_rb.next()
    P.add("dve", lambda e: e.tensor_scalar(out=rb[:], in0=C.ones_f[0:33, :], scalar1=rb_col, scalar2=None, op0=ALU.mult), r=["ones_f", rb_key], w=[rbk])
    g2k = ("g2", slot)
    for cc in range(GW // 512):
        oh, ohk = C.tz_oh.next()
        dma(P, "sp", oh[:], oh_d[:, cc * 512:(cc + 1) * 512], r=[], w=[ohk])
        ps, psk = C.ps[6], C.psk[6]
        P.add("pe", lambda e, oh=oh, ps=ps: e.matmul(ps[:], lhsT=rb[:], rhs=oh[:], start=True, stop=True), r=[rbk, ohk], w=[psk])
        g, gk = C.tz_g.next()
        P.add("act", lambda e, g=g, ps=ps: e.copy(out=g[:], in_=ps[:]), r=[psk], w=[gk])
        dma(P, "act", bass.AP(g2_t, slot * 128 * GW + cc * 512, [[GW, 128], [1, 512]]), g[:], r=[gk], w=[(g2k, cc)])


def emit_toeplitz_load(C, g2_t, slot, width=TW):
    nc, P = C.nc, C.P
    if not hasattr(C, "tz_st"):
        C.tz_st = Rot(nc, "tz_st", 2, [128, 560], F32)
        C.tz_i = 0
    g2k = ("g2", slot)
    T = C.tz_T[C.tz_i % 2]
    Tk = ("tz_T", C.tz_i % 2)
    C.tz_i += 1
    nst = width // 560
    for m in range(nst):
        st, sk = C.tz_st.next()
        dma(P, "sp", st[:], bass.AP(g2_t, slot * 128 * GW + 127 + m * 560, [[GW - 1, 128], [1, 560]]),
            r=[(g2k, cc) for cc in range(GW // 512)], w=[sk])
        P.add("act", lambda e, st=st, m=m: e.activation(out=T[:, m * 560:(m + 1) * 560], in_=st[:], func=AF.Exp), r=[sk], w=[(Tk, m)] + list(C.tz_alias_keys))
    return T, [(Tk, m) for m in range(nst)]


def emit_attention(C, mode, QT, KT, qk_keys, V_of, v_keys, scale, oT_dst, T=None, T_keys=(), negmT=None, negm_key=None, cmask=None, dv=64, bp=None):
    nc, P = C.nc, C.P
    if not hasattr(C, "at_p"):
        C.at_p = Rot(nc, "at_p", 4, [128, 512], BF16)
        C.at_rec = Rot(nc, "at_rec", 2, [65, 512], F32)
        C.at_bc = Rot(nc, "at_bc", 2, [64, 512], F32)
        C.at_o = Rot(nc, "at_o", 2, [128, 512], BF16)
        C.at_z = {0: Rot(nc, "at_z0", 2, [128, 512], BF16), 64: Rot(nc, "at_z64", 2, [128, 512], BF16)}
        for b_, rot_ in C.at_z.items():
            for j_, t_ in enumerate(rot_.tiles):
                P.add("pool", lambda e, t_=t_, b_=b_: e.memset(t_[64 - b_:128 - b_, :], 0.0), r=[], w=[(rot_.name, j_)])
        C.at_sbank = 0
        C.at_obank = 0
    if mode == "sb" and not hasattr(C, "sb_e"):
        C.sb_e = Rot(nc, "sb_e", 2, [128, 512], F32)
        C.sb_sp = Rot(nc, "sb_sp", 2, [128, 512], BF16)
        C.sb_arg = Rot(nc, "sb_arg", 3, [128, 512], F32)
        C.sb_B = salloc(nc, "sb_B", [128, 512], F32)
        C.sb_negU = salloc(nc, "sb_negU", [128, 128], BF16)
        C.sb_negO = salloc(nc, "sb_negO", [128, 128], BF16)
        C.sb_zero = salloc(nc, "sb_zero", [128, 512], BF16)
        P.add("pool", lambda e: e.memset(C.sb_negO[:], -1.0), r=[], w=["sb_negO"])
        P.add("pool", lambda e: e.memset(C.sb_zero[:], 0.0), r=[], w=["sb_zero"])
        P.add("dve", lambda e: e.tensor_copy(out=C.sb_negU[:], in_=C.negU_f[:]), r=["negU_f"], w=["sb_negU"])
    for qg in range(8):
        t0 = qg * 512
        kt_hi = 4 * qg + 3
        kt_lo = max(0, (t0 - 2048) // 128) if mode == "dil" else 0
        kts = list(range(kt_lo, kt_hi + 1))
        if mode == "sb":
            kts = kts[::-1]
        ob = 4 + (C.at_obank % 2)
        C.at_obank += 1
        o_ps, o_psk = C.ps[ob], C.psk[ob]
        nrow = dv + (0 if mode == "sb" else 1)
        if bp is not None:
            Z, Zk = C.at_z[bp].next()
            P.add("pool", lambda e, Z=Z, t0=t0: e.tensor_copy(out=Z[bp:bp + 64, :], in_=QT[:, t0:t0 + 512]), r=list(qk_keys), w=[Zk])
            qkeys_g = [Zk] + list(qk_keys)
        else:
            Z, qkeys_g = None, list(qk_keys)
        if mode == "sb":
            P.add("pe", lambda e, o_ps=o_ps: e.matmul(o_ps[0:128, :], lhsT=C.sb_zero[:, 0:128], rhs=C.sb_zero[:], start=True, stop=False, skip_group_check=True),
                  r=["sb_zero"], w=[o_psk])
            P.add("pool", lambda e: e.memset(C.sb_B[:], 0.0), r=[], w=["sb_B"])

        def scores(kt):
            sb_ = C.at_sbank % 4
            C.at_sbank += 1
            s_ps, s_psk = C.ps[sb_], C.psk[sb_]
            s0 = kt * 128
            c0 = max(0, s0 - t0)
            diag = s0 + 127 > t0 + c0
            ops = []
            need_bias = False
            need_cm = mode in ("mla", "sb") and diag
            c1 = None
            if mode == "moba":
                n = kt // 2
                c1 = max(c0, (n + 1) * 256 - t0)
                if c1 >= 512:
                    c1 = None

            def f(e, t0=t0, Z=Z):
                last = not (need_bias or need_cm or c1 is not None)
                rq = Z[:, c0:512] if Z is not None else QT[:, t0 + c0:t0 + 512]
                ins = e.matmul(s_ps[:, c0:512], lhsT=KT[:, s0:s0 + 128], rhs=rq, start=True, stop=last, skip_group_check=True)
                if need_bias:
                    off = (t0 - s0) + 384
                    last = c1 is None
                    ins = e.matmul(s_ps[:, c0:512], lhsT=C.ident_b[:], rhs=T[:, off + c0:off + 512], start=False, stop=last, skip_group_check=True)
                if need_cm:
                    ins = e.matmul(s_ps[:, c0:c0 + 128], lhsT=C.ident_b[:], rhs=cmask[:], start=False, stop=True, skip_group_check=True)
                if c1 is not None:
                    ins = e.matmul(s_ps[:, c1:512], lhsT=C.Emat[:, kt // 2, :], rhs=negmT[:, t0 + c1:t0 + 512], start=False, stop=True, skip_group_check=True)
                return ins
            rk = list(qkeys_g) + (list(T_keys) if need_bias else []) + (["cmask"] if need_cm else []) + ([negm_key, "Emat"] if c1 is not None else []) + ["ident_b"]
            P.add("pe", f, r=rk, w=[s_psk])
            return (s_ps, s_psk, c0, s0)

        nk = len(kts)
        pend = [scores(kts[0])]
        if nk > 1:
            pend.append(scores(kts[1]))
        prev2 = None
        for i, kt in enumerate(kts):
            if i + 2 < nk:
                pend.append(scores(kts[i + 2]))
            cur = pend.pop(0)
            s_ps, s_psk, c0, s0 = cur
            pT, pTk = C.at_p.next()
            first, lastk = (i == 0), (i == nk - 1)
            if mode != "sb":
                P.add("act", lambda e, pT=pT, s_ps=s_ps, c0=c0: e.activation(out=pT[:, c0:512], in_=s_ps[:, c0:512], func=AF.Exp, scale=scale), r=[s_psk], w=[pTk])
                if mode in ("moba", "dil"):
                    off = (t0 - s0) + 384
                    P.add("dve", lambda e, pT=pT, c0=c0, off=off: e.tensor_tensor(out=pT[:, c0:512], in0=pT[:, c0:512], in1=T[:, off + c0:off + 512], op=ALU.mult),
                          r=[pTk] + list(T_keys), w=[pTk])
                P.add("pe", lambda e, pT=pT, c0=c0, kt=kt, first=first, lastk=lastk, o_ps=o_ps: e.matmul(
                    o_ps[0:nrow, c0:512], lhsT=V_of(kt), rhs=pT[:, c0:512], start=first, stop=lastk, skip_group_check=True),
                    r=[pTk] + list(v_keys), w=[o_psk])
            else:
                E, Ek = C.sb_e.next()
                SP, SPk = C.sb_sp.next()
                ARG, ARGk = C.sb_arg.next()
                P.add("act", lambda e, E=E, s_ps=s_ps, c0=c0: e.activation(out=E[:, c0:512], in_=s_ps[:, c0:512], func=AF.Exp), r=[s_psk], w=[Ek])
                P.add("act", lambda e, E=E, SP=SP, c0=c0: e.activation(out=SP[:, c0:512], in_=E[:, c0:512], func=AF.Ln, bias=C.one_col[:], scale=1.0), r=[Ek, "one_col"], w=[SPk])
                w_ps, w_psk = C.ps[6], C.psk[6]

                def fw(e, SP=SP, c0=c0, s0=s0, w_ps=w_ps, t0=t0, Z=Z):
                    rq = Z[:, c0:512] if Z is not None else QT[:, t0 + c0:t0 + 512]
                    e.matmul(w_ps[:, c0:512], lhsT=KT[:, s0:s0 + 128], rhs=rq, start=True, stop=False, skip_group_check=True)
                    ins = e.matmul(w_ps[:, c0:512], lhsT=C.sb_negU[:], rhs=SP[:, c0:512], start=False, stop=not (s0 + 127 > t0 + c0), skip_group_check=True)
                    if s0 + 127 > t0 + c0:
                        ins = e.matmul(w_ps[:, c0:c0 + 128], lhsT=C.ident_b[:], rhs=cmask[:], start=False, stop=True, skip_group_check=True)
                    return ins
                P.add("pe", fw, r=list(qkeys_g) + [SPk, "sb_negU", "cmask", "ident_b"], w=[w_psk])
                P.add("dve", lambda e, ARG=ARG, w_ps=w_ps, c0=c0: e.tensor_tensor(out=ARG[:, c0:512], in0=w_ps[:, c0:512], in1=C.sb_B[:, c0:512], op=ALU.add),
                      r=[w_psk, "sb_B"], w=[ARGk])
                if not lastk:
                    c_ps, c_psk = C.ps[7], C.psk[7]
                    P.add("pe", lambda e, SP=SP, c0=c0, c_ps=c_ps: e.matmul(c_ps[:, c0:512], lhsT=C.sb_negO[:], rhs=SP[:, c0:512], start=True, stop=True),
                          r=[SPk, "sb_negO"], w=[c_psk])
                    P.add("dve", lambda e, c_ps=c_ps, c0=c0: e.tensor_tensor(out=C.sb_B[:, c0:512], in0=c_ps[:, c0:512], in1=C.sb_B[:, c0:512], op=ALU.add),
                          r=[c_psk, "sb_B"], w=["sb_B"])

                def stage2(pT=pT, pTk=pTk, ARG=ARG, ARGk=ARGk, c0=c0, kt=kt, lastk=lastk, o_ps=o_ps):
                    P.add("act", lambda e: e.activation(out=pT[:, c0:512], in_=ARG[:, c0:512], func=AF.Exp), r=[ARGk], w=[pTk])
                    P.add("pe", lambda e: e.matmul(o_ps[0:128, c0:512], lhsT=V_of(kt), rhs=pT[:, c0:512], start=False, stop=lastk, skip_group_check=True),
                          r=[pTk] + list(v_keys), w=[o_psk])
                if prev2 is not None:
                    prev2()
                prev2 = stage2
        if prev2 is not None:
            prev2()
        oT, oTk = C.at_o.next()
        orow = 0
        if mode == "sb":
            orow = bp
            P.add("act", lambda e, oT=oT, o_ps=o_ps: e.copy(out=oT[bp:bp + 64, :], in_=o_ps[bp:bp + 64, :]), r=[o_psk], w=[oTk])
        else:
            rec, reck = C.at_rec.next()
            P.add("dve", lambda e, rec=rec, o_ps=o_ps: e.reciprocal(out=rec[64:65, :], in_=o_ps[64:65, :]), r=[o_psk], w=[reck])
            b_ps, b_psk = C.ps[7], C.psk[7]
            P.add("pe", lambda e, rec=rec, b_ps=b_ps: e.matmul(b_ps[0:64, :], lhsT=C.ones_f[64:65, 0:64], rhs=rec[64:65, :], start=True, stop=True),
                  r=[reck, "ones_f"], w=[b_psk])
            bc, bck = C.at_bc.next()
            P.add("act", lambda e, bc=bc, b_ps=b_ps: e.copy(out=bc[:], in_=b_ps[0:64, :]), r=[b_psk], w=[bck])
            P.add("dve", lambda e, oT=oT, o_ps=o_ps, bc=bc: e.tensor_tensor(out=oT[0:64, :], in0=o_ps[0:64, :], in1=bc[:], op=ALU.mult), r=[o_psk, bck], w=[oTk])
        dst, dk = oT_dst(t0)
        dma(P, "sp", dst, oT[orow:orow + 64, :], r=[oTk], w=[dk])


def load_cast(C, dst_ap, src_d, shape, key, q="pool", eng="pool"):
    nc, P = C.nc, C.P
    nm = "lc_" + "x".join(str(v) for v in shape)
    if not hasattr(C, nm):
        setattr(C, nm, Rot(nc, nm, 1 if shape[0] == 128 and len(shape) == 3 else 2, list(shape), F32))
    st, sk = getattr(C, nm).next()
    dma(P, q, st[:], src_d, r=[], w=[sk])
    P.add(eng, lambda e: e.tensor_copy(out=dst_ap, in_=st[:]), r=[sk], w=[key])


def build_attn_even():
    nc = bass.Bass("TRN2", target_bir_lowering=False)
    P = Prog(nc)
    C = setup_common(nc, P)
    din = lambda name, shape, dt=F32: nc.dram_tensor(name, shape, dt, kind="ExternalInput").ap()
    hT_all = [din("hT_allA", [2, 512, 2048], BF16), din("hT_allB", [2, 512, 2048], BF16)]
    w_in_t = din("w_in_t", [12, 128, 8, 128])
    relb = din("relb33", [33, 8])
    oh_d = din("onehot", [2, 33, GW])
    emat_d = din("emat", [128, 16 * 128], BF16)
    oT_src = nc.dram_tensor("oT_src", [512, S], BF16, kind="ExternalOutput").ap()
    g2 = nc.dram_tensor("g2", [8 * 128 * GW], F32)
    import os
    if os.environ.get("DBG"):
        dout = lambda name, shape, dt=F32: nc.dram_tensor(name, shape, dt, kind="ExternalOutput").ap()
        C.dbg = {"T": dout("dbg_T", [128, TW], BF16), "negmT": dout("dbg_negmT", [16, S], BF16), "QT": dout("dbg_QT", [128, S], BF16),
                 "KT": dout("dbg_KT", [128, S], BF16), "V": dout("dbg_V", [128, 32 * 8 * 65], BF16)}
        C.dbg_head = int(os.environ["DBG"])
    emit_attn_even(C, hT_all, w_in_t, relb, oh_d, emat_d, oT_src, g2, din("gmask", [128, 512]))
    P.emit()
    return nc


def emit_attn_even(C, hT_all, w_in_t, relb, oh_d, emat_d, oT_src, g2, gmask_d, build_g2=True):
    nc, P = C.nc, C.P
    if not hasattr(C, "win_b"):
        scr = salloc(nc, "scr_e", [128, 8 * 1536], BF16)
        C.win_b = scr[:].rearrange("p (k n) -> p k n", k=8)
        C.tz_T = [scr[:, 0:TW], scr[:, TW:2 * TW]]
        C.tz_alias_keys = [("win_b", i) for i in range(12)]
        C.QT = [salloc(nc, f"QT{i}", [128, S], BF16) for i in range(4)]
        C.KT = [salloc(nc, f"KT{i}", [128, S], BF16) for i in range(4)]
        C.Vaug = salloc(nc, "Vaug", [128, 32, 8, 65], BF16)
        C.hTa = Rot(nc, "hTa", 2, [128, 8, 512], BF16)
        C.Emat = salloc(nc, "Emat", [128, 16, 128], BF16)
        dma(P, "sp", C.Emat[:].rearrange("p a b -> p (a b)"), emat_d, r=[], w=["Emat"])
        C.relb = salloc(nc, "relb", [33, 8], F32)
        C.negmT_rot = Rot(nc, "negmT", 2, [128, S], BF16)
        for j_, t_ in enumerate(C.negmT_rot.tiles):
            P.add("pool", lambda e, t_=t_: e.memset(t_[:, :], 0.0), r=[], w=[("negmT", j_)])
        C.gG = salloc(nc, "gG", [128, 512], F32)
        C.gG2 = salloc(nc, "gG2", [128, 512], F32)
        C.gEQ = salloc(nc, "gEQ", [128, 512], F32)
        C.gm3 = salloc(nc, "gm3", [128, 32], F32)
        C.gmask = salloc(nc, "gmask", [128, 512], F32)
        dma(P, "sp", C.gmask[:], gmask_d, r=[], w=["gmask"])
        C.kms = salloc(nc, "kms", [128, 16], F32)
        C.kmb = salloc(nc, "kmb", [128, 16], BF16)
        P.add("pool", lambda e: e.memset(C.Vaug[:, :, :, 64:65], 1.0), r=[], w=["Vones"])
    dma(P, "sp", C.relb[:], relb, r=[], w=["relb"])
    if build_g2:
        for hi in range(8):
            emit_toeplitz_build(C, g2, hi, C.relb[:, hi:hi + 1], "relb", oh_d[0 if hi < 4 else 1])
    for i in range(12):
        load_cast(C, C.win_b[:, :, i * 128:(i + 1) * 128], w_in_t[i], [128, 8, 128], ("win_b", i))
    wk_all = [("win_b", i) for i in range(12)]
    bank = 0
    for tg in range(8):
        hT, hTk = C.hTa.next()
        for hp in range(2):
            dma(P, "sp", hT[:, 4 * hp:4 * hp + 4, :], hT_all[hp][tg // 4, :, (tg % 4) * 512:(tg % 4 + 1) * 512].rearrange("(k p) n -> p k n", p=128), r=["hT_all"], w=[(hTk, hp)])
        for fo in range(8):
            ps, psk = C.ps[bank % 4], C.psk[bank % 4]
            bank += 1

            def f(e, fo=fo, ps=ps, hT=hT):
                ins = None
                for k in range(8):
                    ins = e.matmul(ps[:], lhsT=C.win_b[:, k, fo * 128:(fo + 1) * 128], rhs=hT[:, k, :], start=(k == 0), stop=(k == 7))
                return ins
            P.add("pe", f, r=[(hTk, 0), (hTk, 1), ("win_b", fo)], w=[psk])
            if fo < 4:
                P.add("act", lambda e, fo=fo, ps=ps, tg=tg: e.activation(out=C.QT[fo][:, tg * 512:(tg + 1) * 512], in_=ps[:], func=AF.Copy, scale=0.125),
                      r=[psk], w=[("QT", fo, tg)])
            else:
                P.add("dve", lambda e, fo=fo, ps=ps, tg=tg: e.tensor_copy(out=C.KT[fo - 4][:, tg * 512:(tg + 1) * 512], in_=ps[:]), r=[psk], w=[("KT", fo - 4, tg)])
        for tt in range(4):
            ps, psk = C.ps[bank % 4], C.psk[bank % 4]
            bank += 1

            def f(e, tt=tt, ps=ps, hT=hT):
                ins = None
                for k in range(8):
                    ins = e.matmul(ps[:], lhsT=hT[:, k, tt * 128:(tt + 1) * 128], rhs=C.win_b[:, k, 1024:1536], start=(k == 0), stop=(k == 7))
                return ins
            P.add("pe", f, r=[(hTk, 0), (hTk, 1)] + wk_all[8:12], w=[psk])
            P.add("act", lambda e, ps=ps, tg=tg, tt=tt: e.copy(out=C.Vaug[:, tg * 4 + tt, :, 0:64], in_=ps[:].rearrange("p (h d) -> p h d", h=8)),
                  r=[psk], w=[("V", tg * 4 + tt)])
    vkeys = [("V", i) for i in range(32)] + ["Vones"]
    heads = []
    for hi in range(8):
        pair, bp = hi // 2, (hi % 2) * 64
        heads.append((hi, pair, bp, C.QT[pair][bp:bp + 64, :], C.KT[pair][bp:bp + 64, :],
                      [("QT", pair, tg) for tg in range(8)] + [("KT", pair, tg) for tg in range(8)]))

    def gating(hi, pair, bp, QTh, KTh, qk_keys):
        negmT, nmk = C.negmT_rot.next()
        P.add("dve", lambda e: e.tensor_reduce(out=C.kms[bp:bp + 64, :], in_=KTh.rearrange("p (n l) -> p n l", l=256), axis=AX.X, op=ALU.add),
              r=qk_keys[8:], w=["kms"])
        P.add("dve", lambda e: e.tensor_scalar(out=C.kmb[bp:bp + 64, :], in0=C.kms[bp:bp + 64, :], scalar1=1.0 / 256, scalar2=None, op0=ALU.mult),
              r=["kms"], w=["kmb"])
        g_ps, g_psk = C.ps[6], C.psk[6]

        def fg(e):
            ins = None
            for qt in range(32):
                ins = e.matmul(g_ps[:, qt * 16:(qt + 1) * 16], lhsT=QTh[:, qt * 128:(qt + 1) * 128], rhs=C.kmb[bp:bp + 64, :], start=True, stop=True)
            return ins
        P.add("pe", fg, r=qk_keys[:8] + ["kmb"], w=[g_psk])
        G, G2, EQ, m = C.gG, C.gG2, C.gEQ, C.gm3
        v3 = lambda t: t[:].rearrange("p (q n) -> p q n", n=16)
        bc = lambda t: t[:].unsqueeze(2).to_broadcast([128, 32, 16])
        P.add("dve", lambda e: e.tensor_tensor(out=G[:], in0=g_ps[:], in1=C.gmask[:], op=ALU.add), r=[g_psk, "gmask"], w=["gG"])
        src, srck = G, "gG"
        for it in range(2):
            P.add("dve", lambda e, src=src: e.tensor_reduce(out=m[:], in_=v3(src), axis=AX.X, op=ALU.max), r=[srck], w=["gm3"])
            P.add("dve", lambda e, src=src: e.tensor_tensor(out=v3(EQ), in0=v3(src), in1=bc(m), op=ALU.is_ge), r=[srck, "gm3"], w=["gEQ"])
            P.add("dve", lambda e, src=src: e.scalar_tensor_tensor(out=G2[:], in0=EQ[:], scalar=-3e30, in1=src[:], op0=ALU.mult, op1=ALU.add),
                  r=["gEQ", srck], w=["gG2"])
            src, srck = G2, "gG2"
        P.add("dve", lambda e: e.tensor_reduce(out=m[:], in_=v3(G2), axis=AX.X, op=ALU.max), r=["gG2"], w=["gm3"])
        P.add("dve", lambda e: e.tensor_scalar(out=m[:], in0=m[:], scalar1=-1e29, scalar2=None, op0=ALU.max), r=["gm3"], w=["gm3"])
        P.add("dve", lambda e: e.tensor_tensor(out=v3(EQ), in0=v3(G), in1=bc(m), op=ALU.is_ge), r=["gG", "gm3"], w=["gEQ"])
        P.add("dve", lambda e: e.tensor_scalar(out=EQ[:], in0=EQ[:], scalar1=-1.0, scalar2=None, op0=ALU.add), r=["gEQ"], w=["gEQ"])
        for grp in range(8):
            t_ps, t_psk = C.ps[7], C.psk[7]
            q0 = 2 if grp == 0 else 0

            def ft(e, grp=grp, q0=q0, t_ps=t_ps):
                ins = None
                for j in range(q0, 4):
                    qt = grp * 4 + j
                    ins = e.transpose(t_ps[0:16, j * 128:(j + 1) * 128], EQ[:, qt * 16:(qt + 1) * 16], C.ident_f[:])
                return ins
            P.add("pe", ft, r=["gEQ", "ident_f"], w=[t_psk])
            P.add("act", lambda e, grp=grp, q0=q0, t_ps=t_ps: e.copy(out=negmT[0:16, grp * 512 + q0 * 128:(grp + 1) * 512], in_=t_ps[0:16, q0 * 128:512]),
                  r=[t_psk], w=[nmk])
        return negmT, nmk

    nxtT = emit_toeplitz_load(C, g2, 0)
    nxtG = gating(*heads[0])
    for (hi, pair, bp, QTh, KTh, qk_keys) in heads:
        is_a = hi < 4
        T, Tkeys = nxtT
        negmT, nmk = nxtG if is_a else (None, None)
        if hi + 1 < 8:
            nxtT = emit_toeplitz_load(C, g2, hi + 1)
            if hi + 1 < 4:
                nxtG = gating(*heads[hi + 1])

        def oT_dst(t0, hi=hi):
            return oT_src[hi * 64:(hi + 1) * 64, t0:t0 + 512], "oT_src"
        emit_attention(C, "moba" if is_a else "dil", QTh, C.KT[pair][:, :], qk_keys, lambda kt, hi=hi: C.Vaug[:, kt, hi, :], vkeys, 1.0, oT_dst,
                       T=T, T_keys=Tkeys, negmT=negmT, negm_key=nmk, bp=bp)


def gate_mask_const():
    m = np.zeros((32, 16), np.float32)
    for qt in range(32):
        m[qt, qt // 2:] = -1e30
    return np.ascontiguousarray(np.broadcast_to(m.reshape(1, 512), (128, 512)))


def prep_even_core(ab_w_in, rel_bias, half):
    w = np.asarray(ab_w_in, np.float32)
    qa, ka, va, qb, kb, vb = [w[:, i * 512:(i + 1) * 512] for i in range(6)]
    hs = slice(half * 256, (half + 1) * 256)
    cols = np.concatenate([qa[:, hs], qb[:, hs], ka[:, hs], kb[:, hs], va[:, hs], vb[:, hs]], axis=1)
    w_in_t = np.ascontiguousarray(cols.reshape(8, 128, 12, 128).transpose(2, 1, 0, 3))
    rb = np.asarray(rel_bias, np.float32)
    own = np.concatenate([rb[4 * half:4 * half + 4], rb[8 + 4 * half:8 + 4 * half + 4]], 0)
    relb33 = np.concatenate([own.T, np.ones((1, 8), np.float32)], 0)
    return {"w_in_t": w_in_t, "relb33": np.ascontiguousarray(relb33)}


def emat_const():
    e = np.zeros((128, 16, 128), np.float32)
    for n in range(16):
        e[n, n, :] = -NEG
    return e.reshape(128, 2048).astype(NPBF)


def build_attn_odd():
    nc = bass.Bass("TRN2", target_bir_lowering=False)
    P = Prog(nc)
    C = setup_common(nc, P)
    din = lambda name, shape, dt=F32: nc.dram_tensor(name, shape, dt, kind="ExternalInput").ap()
    T = {"hT_all": [din("hT_allA", [2, 512, 2048], BF16), din("hT_allB", [2, 512, 2048], BF16)], "w_in_t": din("w_in_t", [12, 128, 8, 128]), "wuq_t": din("wuq_t", [128, 2, 768]),
         "wukv_t": din("wukv_t", [128, 2, 768]), "qkvg": din("qkvg", [128, 4]), "cos_t": din("cos_t", [64, S]), "sin_t": din("sin_t", [64, S]),
         "cmasks": din("cmasks", [128, 2, 128]), "negU": din("negU", [128, 128])}
    oT_src = nc.dram_tensor("oT_src", [512, S], BF16, kind="ExternalOutput").ap()
    emit_attn_odd(C, T, oT_src)
    print("odd SBUF peak", C.A.peak)
    P.emit()
    return nc


def emit_attn_odd(C, T, oT_src):
    nc, P = C.nc, C.P
    hT_all = T["hT_all"]
    cm = salloc(nc, "cm", [128, 2, 128], BF16)
    load_cast(C, cm[:], T["cmasks"], [128, 2, 128], "cmask", q="sp", eng="dve")
    C.negU_f = salloc(nc, "negU_f", [128, 128], F32)
    dma(P, "sp", C.negU_f[:], T["negU"], r=[], w=["negU_f"])
    ones_b = salloc(nc, "ones_b", [128, 128], BF16)
    P.add("pool", lambda e: e.memset(ones_b[:], 1.0), r=[], w=["ones_b"])
    hTa = Rot(nc, "hTa", 2, [128, 8, 512], BF16)
    win_b = salloc(nc, "win_b", [128, 8, 768], BF16)
    C0 = C
    C = scope_open(C0)
    for i in range(6):
        load_cast(C, win_b[:, :, i * 128:(i + 1) * 128], T["w_in_t"][i], [128, 8, 128], ("win_b", i))
    wuq_b = salloc(nc, "wuq_b", [128, 2, 768], BF16)
    wukv_b = salloc(nc, "wukv_b", [128, 2, 768], BF16)
    for ch in range(2):
        load_cast(C, wuq_b[:, ch, :], T["wuq_t"][:, ch, :], [128, 768], ("wuq_b", ch))
        load_cast(C, wukv_b[:, ch, :], T["wukv_t"][:, ch, :], [128, 768], ("wukv_b", ch))
    wuqk = [("wuq_b", 0), ("wuq_b", 1)]
    wukvk = [("wukv_b", 0), ("wukv_b", 1)]
    gcol = salloc(nc, "qkvg", [128, 4], F32)
    dma(P, "sp", gcol[:], T["qkvg"], r=[], w=["qkvg"])
    QTc = [salloc(nc, f"QTc{i}", [128, S], BF16) for i in range(4)]
    KTc = [salloc(nc, f"KTc{i}", [128, S], BF16) for i in range(4)]
    Vc = salloc(nc, "Vc", [128, 32, 4, 65], BF16)
    P.add("pool", lambda e: e.memset(Vc[:, :, :, 64:65], 1.0), r=[], w=["Vones"])
    raw = Rot(nc, "raw", 4, [128, 512], F32)
    sq = Rot(nc, "sq", 4, [128, 512], BF16)
    rstd = Rot(nc, "rstdb", 2, [128, 512], F32)
    cn = Rot(nc, "cn", 2, [128, 4, 512], BF16)
    cs = Rot(nc, "cs", 2, [64, 2, 512], F32)
    t1 = Rot(nc, "rt1", 2, [64, 512], F32)
    t2 = Rot(nc, "rt2", 2, [64, 512], F32)
    rb = [0]

    def nbank():
        b = rb[0] % 6
        rb[0] += 1
        return C.ps[b], C.psk[b]

    def mmk(ps, lhs_of, rhs_of, n, rows=None, cols=None):
        def f(e):
            ins = None
            for k in range(n):
                out = ps[:] if rows is None else ps[rows[0]:rows[1], cols[0]:cols[1]]
                ins = e.matmul(out, lhsT=lhs_of(k), rhs=rhs_of(k), start=(k == 0), stop=(k == n - 1))
            return ins
        return f

    def rope(pre_ps, pre_k, sw_ps, sw_k, cst, csk, outs):
        a, ak = t1.next()
        b, bk = t2.next()
        P.add("dve", lambda e: e.tensor_tensor(out=a[:], in0=pre_ps[0:64, :], in1=cst[:, 0, :], op=ALU.mult), r=[pre_k] + list(csk), w=[ak])
        P.add("dve", lambda e: e.tensor_tensor(out=b[:], in0=sw_ps[0:64, :], in1=cst[:, 1, :], op=ALU.mult), r=[sw_k] + list(csk), w=[bk])
        (o0, k0) = outs[0]
        P.add("pool", lambda e: e.tensor_tensor(out=o0, in0=a[:], in1=b[:], op=ALU.add), r=[ak, bk], w=[k0])
        for (o, k) in outs[1:]:
            P.add("pool", lambda e, o=o: e.tensor_copy(out=o, in_=o0), r=[k0], w=[k])

    for tg in range(8):
        cols = slice(tg * 512, (tg + 1) * 512)
        hT, hTk = hTa.next()
        for hp in range(2):
            dma(P, "sp", hT[:, 4 * hp:4 * hp + 4, :], hT_all[hp][tg // 4, :, (tg % 4) * 512:(tg % 4 + 1) * 512].rearrange("(k p) n -> p k n", p=128), r=["hT_all"], w=[(hTk, hp)])
        cst, csk = cs.next()
        dma(P, "sp", cst[:, 0, :], T["cos_t"][:, cols], r=[], w=[(csk, 0)])
        dma(P, "sp", cst[:, 1, :], T["sin_t"][:, cols], r=[], w=[(csk, 1)])
        cskk = [(csk, 0), (csk, 1)]
        raws, sqs = [], []
        for ch in range(4):
            ps, psk = nbank()
            P.add("pe", mmk(ps, lambda k, ch=ch: win_b[:, k, ch * 128:(ch + 1) * 128], lambda k, hT=hT: hT[:, k, :], 8), r=[(hTk, 0), (hTk, 1), ("win_b", ch)], w=[psk])
            r_, rk = raw.next()
            s_, sk = sq.next()
            P.add("act", lambda e, r_=r_, ps=ps: e.copy(out=r_[:], in_=ps[:]), r=[psk], w=[rk])
            P.add("act", lambda e, s_=s_, ps=ps: e.activation(out=s_[:], in_=ps[:], func=AF.Square), r=[psk], w=[sk])
            raws.append((r_, rk))
            sqs.append((s_, sk))
        cnt, cnk = cn.next()
        for which in range(2):
            ss_ps, ss_k = C.ps[6 + which], C.psk[6 + which]
            (sa, sak), (sb2, sbk) = sqs[2 * which], sqs[2 * which + 1]
            P.add("pe", lambda e, ss_ps=ss_ps, sa=sa, sb2=sb2: (e.matmul(ss_ps[:], lhsT=ones_b[:], rhs=sa[:], start=True, stop=False),
                                                               e.matmul(ss_ps[:], lhsT=ones_b[:], rhs=sb2[:], start=False, stop=True))[1],
                  r=[sak, sbk, "ones_b"], w=[ss_k])
            rs, rsk = rstd.next()
            P.add("act", lambda e, rs=rs, ss_ps=ss_ps: e.activation(out=rs[:], in_=ss_ps[:], func=AF.Ln, bias=C.eps_col[:], scale=1.0 / 256), r=[ss_k, "eps"], w=[rsk])
            P.add("act", lambda e, rs=rs: e.activation(out=rs[:], in_=rs[:], func=AF.Exp, scale=-0.5), r=[rsk], w=[rsk])
            for c2 in range(2):
                ch = 2 * which + c2
                (r_, rk) = raws[ch]
                P.add("dve", lambda e, r_=r_, ch=ch, rs=rs, cnt=cnt: e.scalar_tensor_tensor(out=cnt[:, ch, :], in0=r_[:], scalar=gcol[:, ch:ch + 1], in1=rs[:],
                                                                                         op0=ALU.mult, op1=ALU.mult), r=[rk, rsk, "qkvg"], w=[(cnk, ch)])
        cqk = [(cnk, 0), (cnk, 1)]
        ckvk = [(cnk, 2), (cnk, 3)]
        kr_ps, kr_k = nbank()
        P.add("pe", mmk(kr_ps, lambda k: win_b[:, k, 512:576], lambda k, hT=hT: hT[:, k, :], 8, rows=(0, 64), cols=(0, 512)), r=[(hTk, 0), (hTk, 1), ("win_b", 4)], w=[kr_k])
        krs_ps, krs_k = nbank()
        P.add("pe", mmk(krs_ps, lambda k: win_b[:, k, 640:704], lambda k, hT=hT: hT[:, k, :], 8, rows=(0, 64), cols=(0, 512)), r=[(hTk, 0), (hTk, 1), ("win_b", 5)], w=[krs_k])
        rope(kr_ps, kr_k, krs_ps, krs_k, cst, cskk, [(KTc[h][0:64, cols], ("KTc", h, tg, 0)) for h in range(4)])
        for h in range(4):
            pre_ps, pre_k = nbank()
            P.add("pe", mmk(pre_ps, lambda k, h=h: wuq_b[:, k, h * 192:h * 192 + 128], lambda k, cnt=cnt: cnt[:, k, :], 2), r=cqk + wuqk, w=[pre_k])
            sw_ps, sw_k = nbank()
            P.add("pe", mmk(sw_ps, lambda k, h=h: wuq_b[:, k, h * 192 + 128:h * 192 + 192], lambda k, cnt=cnt: cnt[:, k, :], 2, rows=(0, 64), cols=(0, 512)),
                  r=cqk + wuqk, w=[sw_k])
            P.add("act", lambda e, h=h, pre_ps=pre_ps, cols=cols: e.copy(out=QTc[h][64:128, cols], in_=pre_ps[64:128, :]), r=[pre_k], w=[("QTc", h, tg, 1)])
            rope(pre_ps, pre_k, sw_ps, sw_k, cst, cskk, [(QTc[h][0:64, cols], ("QTc", h, tg, 0))])
            kn_ps, kn_k = nbank()
            P.add("pe", mmk(kn_ps, lambda k, h=h: wukv_b[:, k, h * 128:(h + 1) * 128], lambda k, cnt=cnt: cnt[:, 2 + k, :], 2), r=ckvk + wukvk, w=[kn_k])
            P.add("act", lambda e, h=h, kn_ps=kn_ps, cols=cols: e.copy(out=KTc[h][64:128, cols], in_=kn_ps[64:128, :]), r=[kn_k], w=[("KTc", h, tg, 1)])
        for tt in range(4):
            v_ps, v_k = nbank()
            P.add("pe", mmk(v_ps, lambda k, tt=tt, cnt=cnt: cnt[:, 2 + k, tt * 128:(tt + 1) * 128], lambda k: wukv_b[:, k, 512:768], 2, rows=(0, 128), cols=(0, 256)),
                  r=ckvk + wukvk, w=[v_k])
            P.add("act", lambda e, v_ps=v_ps, tg=tg, tt=tt: e.copy(out=Vc[:, tg * 4 + tt, :, 0:64], in_=v_ps[:, 0:256].rearrange("p (h d) -> p h d", h=4)),
                  r=[v_k], w=[("Vc", tg * 4 + tt)])
    vkeys = [("Vc", i) for i in range(32)] + ["Vones"]
    for h in range(4):
        qk_keys = [("QTc", h, tg, j) for tg in range(8) for j in range(2)] + [("KTc", h, tg, j) for tg in range(8) for j in range(2)]
        emit_attention(C, "mla", QTc[h], KTc[h], qk_keys, lambda kt, h=h: Vc[:, kt, h, :], vkeys, 96 ** -0.5,
                       lambda t0, h=h: (oT_src[h * 64:(h + 1) * 64, t0:t0 + 512], "oT_src"), cmask=cm[:, 0, :])
    scope_close(C)
    C = scope_open(C0)
    for i in range(6):
        load_cast(C, win_b[:, :, i * 128:(i + 1) * 128], T["w_in_t"][6 + i], [128, 8, 128], ("win_b", i))
    QTd = [salloc(nc, f"QTd{i}", [128, S], BF16) for i in range(2)]
    KTd = [salloc(nc, f"KTd{i}", [128, S], BF16) for i in range(2)]
    Vd = salloc(nc, "Vd", [128, 32, 4, 64], BF16)
    bank = 0
    for tg in range(8):
        hT, hTk = hTa.next()
        for hp in range(2):
            dma(P, "sp", hT[:, 4 * hp:4 * hp + 4, :], hT_all[hp][tg // 4, :, (tg % 4) * 512:(tg % 4 + 1) * 512].rearrange("(k p) n -> p k n", p=128), r=["hT_all"], w=[(hTk, hp)])
        for fo in range(4):
            ps, psk = C.ps[bank % 4], C.psk[bank % 4]
            bank += 1
            P.add("pe", mmk(ps, lambda k, fo=fo: win_b[:, k, fo * 128:(fo + 1) * 128], lambda k, hT=hT: hT[:, k, :], 8), r=[(hTk, 0), (hTk, 1), ("win_b", fo)], w=[psk])
            if fo < 2:
                P.add("act", lambda e, fo=fo, ps=ps, tg=tg: e.activation(out=QTd[fo][:, tg * 512:(tg + 1) * 512], in_=ps[:], func=AF.Copy, scale=0.125),
                      r=[psk], w=[("QTd", fo, tg)])
            else:
                P.add("dve", lambda e, fo=fo, ps=ps, tg=tg: e.tensor_copy(out=KTd[fo - 2][:, tg * 512:(tg + 1) * 512], in_=ps[:]), r=[psk], w=[("KTd", fo - 2, tg)])
        for tt in range(4):
            ps, psk = C.ps[bank % 4], C.psk[bank % 4]
            bank += 1
            P.add("pe", mmk(ps, lambda k, tt=tt, hT=hT: hT[:, k, tt * 128:(tt + 1) * 128], lambda k: win_b[:, k, 512:768], 8, rows=(0, 128), cols=(0, 256)),
                  r=[(hTk, 0), (hTk, 1), ("win_b", 4), ("win_b", 5)], w=[psk])
            P.add("act", lambda e, ps=ps, tg=tg, tt=tt: e.copy(out=Vd[:, tg * 4 + tt, :, :], in_=ps[:, 0:256].rearrange("p (h d) -> p h d", h=4)),
                  r=[psk], w=[("Vd", tg * 4 + tt)])
    vkeys = [("Vd", i) for i in range(32)]
    for h in range(4):
        pair, bp = h // 2, (h % 2) * 64
        qk_keys = [("QTd", pair, tg) for tg in range(8)] + [("KTd", pair, tg) for tg in range(8)]
        emit_attention(C, "sb", QTd[pair][bp:bp + 64, :], KTd[pair][:, :], qk_keys,
                       lambda kt, pair=pair: Vd[:, kt, 2 * pair:2 * pair + 2, :].rearrange("p a d -> p (a d)"), vkeys, 1.0,
                       lambda t0, h=h: (oT_src[256 + h * 64:256 + (h + 1) * 64, t0:t0 + 512], "oT_src"), cmask=cm[:, 1, :], bp=bp)
    scope_close(C)


def prep_odd_core(cd_w_in, q_g, kv_g, w_uq, w_ukv, half):
    w = np.asarray(cd_w_in, np.float32)
    z16 = np.zeros((1024, 16), np.float32)
    cq, ckv, kr = w[:, 0:256], w[:, 256:512], w[:, 512:544]
    qd, kd, vd = w[:, 544:1056], w[:, 1056:1568], w[:, 1568:2080]
    hs = slice(half * 256, (half + 1) * 256)
    x1, x2 = kr[:, 0:16], kr[:, 16:32]
    z64 = np.zeros((1024, 64), np.float32)
    kr_pre = np.concatenate([x1, z16, x2, z16, z64], 1)
    kr_sw = np.concatenate([x2, z16, x1, z16, z64], 1)
    cols = np.concatenate([cq, ckv, kr_pre, kr_sw, qd[:, hs], kd[:, hs], vd[:, hs]], 1)
    w_in_t = np.ascontiguousarray(cols.reshape(8, 128, 12, 128).transpose(2, 1, 0, 3))
    wq = np.asarray(w_uq, np.float32)
    wkv = np.asarray(w_ukv, np.float32)
    z = np.zeros((256, 16), np.float32)
    qcols, kcols, vcols = [], [], []
    for h in range(4 * half, 4 * half + 4):
        nope, pe = wq[:, h * 96:h * 96 + 64], wq[:, h * 96 + 64:h * 96 + 96]
        p1, p2 = pe[:, 0:16], pe[:, 16:32]
        qcols += [p1, z, p2, z, nope, p2, z, p1, z]
        kcols += [np.zeros((256, 64), np.float32), wkv[:, h * 128:h * 128 + 64]]
        vcols += [wkv[:, h * 128 + 64:h * 128 + 128]]
    wuq = np.concatenate(qcols, 1)
    wukv = np.concatenate(kcols + vcols, 1)
    tile2 = lambda a: np.ascontiguousarray(a.reshape(2, 128, a.shape[1]).transpose(1, 0, 2))
    g = np.concatenate([np.asarray(q_g, np.float32).reshape(2, 128).T, np.asarray(kv_g, np.float32).reshape(2, 128).T], 1)
    return {"w_in_t": w_in_t, "wuq_t": tile2(wuq), "wukv_t": tile2(wukv), "qkvg": np.ascontiguousarray(g)}


def rope_tables():
    inv = (1.0 / (np.float32(10000.0) ** (np.arange(0, 32, 2, dtype=np.float32) / np.float32(32)))).astype(np.float32)
    ang = np.arange(S, dtype=np.float32)[:, None] * inv[None, :]
    c, s_ = np.cos(ang).astype(np.float32).T, np.sin(ang).astype(np.float32).T
    cos_t = np.zeros((64, S), np.float32)
    sin_t = np.zeros((64, S), np.float32)
    cos_t[0:16], cos_t[32:48] = c, c
    sin_t[0:16], sin_t[32:48] = -s_, s_
    return cos_t, sin_t


def sb_negU():
    i = np.arange(128)
    return np.where(i[:, None] >= i[None, :], -1.0, 0.0).astype(np.float32)


_PROGS = {}


def _prog(name, fn):
    if name not in _PROGS:
        _PROGS[name] = fn()
    return _PROGS[name]


def _perm_wout(w):
    w = np.asarray(w, np.float32)
    order = np.concatenate([np.arange(r * 256, (r + 1) * 256) if t == 0 else 512 + np.arange(r * 256, (r + 1) * 256)
                            for r in range(2) for t in range(2)])
    return w[order]


def _run(nc, in_maps):
    return run_bass_kernel_spmd(nc, in_maps, core_ids=list(range(8))).results


def kernel_unfused(x, c, rel_bias, ada_w, ada_b, mix_pre_g, mix_post_g, ffn_pre_g, ffn_post_g,
                   ab_w_in, ab_w_out, cd_w_in, mla_q_norm_g, mla_kv_norm_g, mla_w_uq, mla_w_ukv, cd_w_out,
                   ffn_w_up, ffn_conv_w, ffn_conv_b, ffn_w_down):
    f32 = lambda a: np.asarray(a, np.float32)
    x, c = f32(x), f32(c)
    ident = np.eye(128, dtype=np.float32)
    cores = list(range(8))
    x_core = []
    for cid in cores:
        b, half = cid // 2, cid % 2
        xs = np.zeros((NT * 128, D), np.float32)
        if half == 0:
            xs[128:] = x[b, 0:2048]
        else:
            xs[:] = x[b, 1920:4096]
        x_core.append(xs)
    common = [{"ident_f": ident, "flags": core_flags(cid), "c_col": col_layout(c[cid // 2])} for cid in cores]
    onehot, emat = onehot_tables(), emat_const()
    cos_t, sin_t = rope_tables()
    cmasks = np.ascontiguousarray(causal_masks().transpose(1, 0, 2))
    negU = sb_negU()

    def pre_inputs(l):
        return {"gains_pre": col_layout(mix_pre_g[l]), "ada_w_t_pre": ada_w_tiles(ada_w[l]), "ada_b_col_pre": col_layout(ada_b[l])}

    def post_inputs(l):
        w_out = ab_w_out[l // 2] if l % 2 == 0 else cd_w_out[l // 2]
        return {"w_out_t": k_tiles(_perm_wout(w_out)), "w_down_t": k_tiles(ffn_w_down[l]), "w_up_t": w_up_tiles(ffn_w_up[l]),
                "conv_col": conv_cols(ffn_conv_w[l], ffn_conv_b[l]),
                "gains_post": np.ascontiguousarray(np.stack([col_layout(mix_post_g[l]), col_layout(ffn_pre_g[l]), col_layout(ffn_post_g[l])], 1)),
                "ada_w_t_post": ada_w_tiles(ada_w[l]), "ada_b_col_post": col_layout(ada_b[l])}

    oT_all = None
    for l in range(5):
        do_post, do_pre = l > 0, l < 4
        nc = _prog(("tok", do_post, do_pre), lambda: build_tok(do_post, do_pre))
        shared = {}
        if do_post:
            shared.update(post_inputs(l - 1))
        if do_pre:
            shared.update(pre_inputs(l))
        in_maps = []
        for cid in cores:
            m = dict(common[cid], x_in=x_core[cid], **shared)
            if do_post:
                m["oT_allA"] = np.ascontiguousarray(oT_all[cid // 2][:, 0:256])
                m["oT_allB"] = np.ascontiguousarray(oT_all[cid // 2][:, 256:512])
            in_maps.append(m)
        res = _run(nc, in_maps)
        if do_post:
            x_core = [res[cid]["x_out"] for cid in cores]
        if not do_pre:
            break
        hT_all = [np.ascontiguousarray(np.stack([res[2 * b]["hT_src"], res[2 * b + 1]["hT_src"]], 0)) for b in range(4)]
        if l % 2 == 0:
            nc = _prog("even", build_attn_even)
            per = [prep_even_core(ab_w_in[l // 2], rel_bias, h) for h in range(2)]
            in_maps = [dict(per[cid % 2], ident_f=ident, hT_allA=np.ascontiguousarray(hT_all[cid // 2][:, 0:512]), hT_allB=np.ascontiguousarray(hT_all[cid // 2][:, 512:1024]),
                            onehot=onehot, emat=emat, gmask=gate_mask_const()) for cid in cores]
        else:
            nc = _prog("odd", build_attn_odd)
            i = l // 2
            per = [prep_odd_core(cd_w_in[i], mla_q_norm_g[i], mla_kv_norm_g[i], mla_w_uq[i], mla_w_ukv[i], h) for h in range(2)]
            in_maps = [dict(per[cid % 2], ident_f=ident, hT_allA=np.ascontiguousarray(hT_all[cid // 2][:, 0:512]), hT_allB=np.ascontiguousarray(hT_all[cid // 2][:, 512:1024]),
                            cos_t=cos_t, sin_t=sin_t, cmasks=cmasks, negU=negU) for cid in cores]
        res = _run(nc, in_maps)
        oT_all = [np.ascontiguousarray(np.stack([res[2 * b]["oT_src"], res[2 * b + 1]["oT_src"]], 0)) for b in range(4)]
    out = np.zeros((4, S, D), np.float32)
    for cid in cores:
        b, half = cid // 2, cid % 2
        out[b, half * 2048:(half + 1) * 2048] = x_core[cid][128:]
    return out


def kernel(**inputs):
    return kernel_fused(**inputs)


PAIRS = [[0, 1], [2, 3], [4, 5], [6, 7]]


NLAYERS = [4]


def build_fused():
    NL = NLAYERS[0]
    nc = bass.Bass("TRN2", target_bir_lowering=False)
    P = Prog(nc)
    C = setup_common(nc, P)
    din = lambda name, shape, dt=F32: nc.dram_tensor(name, shape, dt, kind="ExternalInput").ap()
    x_in = din("x_in", [NT * 128, D])
    flags_d = din("flags", [128, 4])
    c_col = din("c_col", [128, 8])
    x_out = nc.dram_tensor("x_out", [NT * 128, D], F32, kind="ExternalOutput").ap()
    x_dram = nc.dram_tensor("x_dram", [NT * 128, D], F32).ap()
    hT_src = nc.dram_tensor("hT_src", [D, 2048], BF16).ap()
    hT_all2 = [nc.dram_tensor(f"hT_all{i}", [2 * 512, 2048], BF16).ap() for i in range(2)]
    oT_src = nc.dram_tensor("oT_src", [512, S], BF16).ap()
    oT_all2 = [nc.dram_tensor(f"oT_all{i}", [2 * 256, S], BF16).ap() for i in range(2)]
    g2 = nc.dram_tensor("g2", [8 * 128 * GW], F32)
    wcache = nc.dram_tensor("wcache", [NCH, 128, 8 * 256], BF16).ap()
    hT_all = [a.rearrange("(r k) n -> r k n", r=2) for a in hT_all2]
    oT_all = [a.rearrange("(r k) n -> r k n", r=2) for a in oT_all2]
    flags = salloc(nc, "flags_sb", [128, 4], F32)
    dma(P, "sp", flags[:], flags_d, r=[], w=["flags"])
    setup_cond(C, c_col)
    mods = [salloc(nc, f"modL{l}", [128, 48], F32) for l in range(4)]
    oh_d = din("onehot", [2, 33, GW])
    emat_d = din("emat", [128, 16 * 128], BF16)
    relb = din("relb33", [33, 8])
    gmask_d = din("gmask", [128, 512])
    odd_const = {"cos_t": din("cos_t", [64, S]), "sin_t": din("sin_t", [64, S]), "cmasks": din("cmasks", [128, 2, 128]), "negU": din("negU", [128, 128])}
    Tq_prev = None
    for l in range(NL + 1):
        do_post, do_pre = l > 0, l < NL
        C0 = C
        C = scope_open(C0)
        Tp = Tq = None
        if do_post:
            lp = l - 1
            Tp = {"w_out_t": din(f"L{lp}_w_out_t", [128, 8, 1024]), "w_down_t": din(f"L{lp}_w_down_t", [128, NCH, 1024]),
                  "w_up_t": din(f"L{lp}_w_up_t", [NCH, 2, 128, 8, 128]), "conv_col": din(f"L{lp}_conv_col", [128, 44, 4]),
                  "gains_post": din(f"L{lp}_gains_post", [128, 3, 8]), "mod": mods[lp], "modk": f"mod_L{lp}", "wcache": wcache}
            tok_setup_post(C, Tp, flags)
        pre_setup = None
        if do_pre:
            Tq = {"gains_pre": din(f"L{l}_gains_pre", [128, 8]), "mod": mods[l], "modk": f"mod_L{l}"}
            adw, adb = din(f"L{l}_ada_w_t", [48, 128, 8, 128]), din(f"L{l}_ada_b_col", [128, 48])

            def pre_setup(C=C, Tq=Tq, adw=adw, adb=adb, l=l):
                emit_mod(C, c_col, adw, adb, f"L{l}", mod=mods[l])
                tok_setup_pre(C, Tq)
        x_src = x_in if l <= 1 else x_dram
        x_dst = x_out if l == NL else x_dram
        emit_tok(C, Tp, Tq, x_src, x_dst, oT_all, hT_src, flags, pre_setup=pre_setup)
        if l == 0:
            relb_t = salloc(nc, "relb0", [33, 8], F32)
            dma(P, "sp", relb_t[:], relb, r=[], w=["relb0"])
            C.tz_oh = Rot(nc, "tz_oh", 4, [33, 512], F32)
            C.tz_rb = Rot(nc, "tz_rb", 2, [33, 128], F32)
            C.tz_g = Rot(nc, "tz_g", 4, [128, 512], F32)
            for hi in range(8):
                emit_toeplitz_build(C, g2, hi, relb_t[:, hi:hi + 1], "relb0", oh_d[0 if hi < 4 else 1])
        print("tok phase", l, "SBUF peak", C.A.peak, flush=True)
        scope_close(C)
        C = C0
        if not do_pre:
            break
        for i in range(2):
            P.add("pool", lambda e, i=i: e.collective_compute("AllGather", ALU.bypass, replica_groups=PAIRS, ins=[hT_src[i * 512:(i + 1) * 512, :]], outs=[hT_all2[i]]),
                  r=["hT_src"], w=["hT_all"], kind="cc")
        C = scope_open(C0)
        if l % 2 == 0:
            emit_attn_even(C, hT_all, din(f"L{l}_w_in_t", [12, 128, 8, 128]), relb, oh_d, emat_d, oT_src, g2, gmask_d, build_g2=False)
        else:
            T = dict(odd_const, hT_all=hT_all, w_in_t=din(f"L{l}_w_in_t", [12, 128, 8, 128]), wuq_t=din(f"L{l}_wuq_t", [128, 2, 768]),
                     wukv_t=din(f"L{l}_wukv_t", [128, 2, 768]), qkvg=din(f"L{l}_qkvg", [128, 4]))
            emit_attn_odd(C, T, oT_src)
        print("attn phase", l, "SBUF peak", C.A.peak, flush=True)
        scope_close(C)
        C = C0
        for i in range(2):
            P.add("pool", lambda e, i=i: e.collective_compute("AllGather", ALU.bypass, replica_groups=PAIRS, ins=[oT_src[i * 256:(i + 1) * 256, :]], outs=[oT_all2[i]]),
                  r=["oT_src"], w=["oT_all"], kind="cc")
    print("fused ops", len(P.ops), flush=True)
    P.emit()
    print("emit stats", sorted(P.stats.items()), flush=True)
    print("instrs per engine", {k: len(list(v.instructions)) if hasattr(v, "instructions") else None for k, v in {}.items()})
    return nc


def fused_inputs(x, c, rel_bias, ada_w, ada_b, mix_pre_g, mix_post_g, ffn_pre_g, ffn_post_g,
                 ab_w_in, ab_w_out, cd_w_in, mla_q_norm_g, mla_kv_norm_g, mla_w_uq, mla_w_ukv, cd_w_out,
                 ffn_w_up, ffn_conv_w, ffn_conv_b, ffn_w_down):
    f32 = lambda a: np.asarray(a, np.float32)
    x, c = f32(x), f32(c)
    shared = {"ident_f": np.eye(128, dtype=np.float32), "onehot": onehot_tables(), "emat": emat_const(), "gmask": gate_mask_const()}
    shared["cos_t"], shared["sin_t"] = rope_tables()
    shared["cmasks"] = np.ascontiguousarray(causal_masks().transpose(1, 0, 2))
    shared["negU"] = sb_negU()
    per_half = [dict(), dict()]
    for l in range(4):
        w_out = ab_w_out[l // 2] if l % 2 == 0 else cd_w_out[l // 2]
        shared[f"L{l}_w_out_t"] = k_tiles(_perm_wout(w_out))
        shared[f"L{l}_w_down_t"] = k_tiles(ffn_w_down[l])
        shared[f"L{l}_w_up_t"] = w_up_tiles(ffn_w_up[l])
        shared[f"L{l}_conv_col"] = conv_cols(ffn_conv_w[l], ffn_conv_b[l])
        shared[f"L{l}_gains_post"] = np.ascontiguousarray(np.stack([col_layout(mix_post_g[l]), col_layout(ffn_pre_g[l]), col_layout(ffn_post_g[l])], 1))
        shared[f"L{l}_gains_pre"] = col_layout(mix_pre_g[l])
        shared[f"L{l}_ada_w_t"] = ada_w_tiles(ada_w[l])
        shared[f"L{l}_ada_b_col"] = col_layout(ada_b[l])
        for h in range(2):
            if l % 2 == 0:
                pe = prep_even_core(ab_w_in[l // 2], rel_bias, h)
                per_half[h][f"L{l}_w_in_t"] = pe["w_in_t"]
                per_half[h]["relb33"] = pe["relb33"]
            else:
                i = l // 2
                po = prep_odd_core(cd_w_in[i], mla_q_norm_g[i], mla_kv_norm_g[i], mla_w_uq[i], mla_w_ukv[i], h)
                for k, v in po.items():
                    per_half[h][f"L{l}_{k}"] = v
    in_maps = []
    for cid in range(8):
        b, half = cid // 2, cid % 2
        xs = np.zeros((NT * 128, D), np.float32)
        if half == 0:
            xs[128:] = x[b, 0:2048]
        else:
            xs[:] = x[b, 1920:4096]
        in_maps.append(dict(shared, **per_half[half], x_in=xs, flags=core_flags(cid), c_col=col_layout(c[b])))
    return in_maps


def kernel_fused(**inputs):
    nc = _prog("fused", build_fused)
    res = _run(nc, fused_inputs(**inputs))
    out = np.zeros((4, S, D), np.float32)
    for cid in range(8):
        b, half = cid // 2, cid % 2
        out[b, half * 2048:(half + 1) * 2048] = res[cid]["x_out"][128:]
    return out
```
